# Optimizing a Trainium2 kernel written in Bass

```python
import jax, jax.numpy as jnp
from jax import lax
import numpy as np

D_MODEL = 1024
BATCH = 32
SEQ = 2048
DEPTH = 1

PLE_DIM = 256
NSA_HEADS = 8
NSA_KV_GROUPS = 2
NSA_HPG = NSA_HEADS // NSA_KV_GROUPS
NSA_HEAD_DIM = 64
Q_DIM = NSA_HEADS * NSA_HEAD_DIM
KV_DIM = NSA_KV_GROUPS * NSA_HEAD_DIM
NSA_BRANCHES = 3
NSA_GATE_DIM = NSA_HEADS * NSA_BRANCHES
CMP_BLOCK = 32
CMP_STRIDE = 16
CMP_HIDDEN = 128
SEL_BLOCK = 64
SEL_TOPK = 16
SEL_Q_CHUNK = 32
WINDOW = 512
WIN_Q_BLOCK = 128
ATTN_SCALE = NSA_HEAD_DIM ** -0.5
FORCE_BONUS = 1e4
NEG_INF = -1e30
CONV_DIM = 512
CONV_WIDTH = 3
IN_PROJ_DIM = Q_DIM + 6 * KV_DIM + NSA_GATE_DIM + 3 * CONV_DIM + 2 * D_MODEL
N_GROUPS = 4
EXPERTS_PER_GROUP = 8
N_EXPERTS = N_GROUPS * EXPERTS_PER_GROUP
EXPERT_HIDDEN = 256
TOPK_EXPERTS = 2
LN_EPS = 1e-5
DEEPNORM_ALPHA = (2.0 * DEPTH) ** 0.25
DEEPNORM_BETA = (8.0 * DEPTH) ** -0.25

kernel_name = "hybrid_nsa_shortconv_hmoe_deepnorm"


def _layer_norm(x, g, b):
    xf = x.astype(jnp.float32)
    mu = jnp.mean(xf, axis=-1, keepdims=True)
    var = jnp.mean(jnp.square(xf - mu), axis=-1, keepdims=True)
    y = (xf - mu) * lax.rsqrt(var + LN_EPS) * g.astype(jnp.float32) + b.astype(jnp.float32)
    return y.astype(x.dtype)


def _compress(kv, pe, w1, b1, w2, b2):
    B, S, G, dk = kv.shape
    n_cmp = (S - CMP_BLOCK) // CMP_STRIDE + 1
    idx = np.arange(n_cmp)[:, None] * CMP_STRIDE + np.arange(CMP_BLOCK)[None, :]
    blk = kv[:, idx] + pe[:, None, :]
    blk = jnp.transpose(blk, (0, 1, 3, 2, 4)).reshape(B, n_cmp, G, CMP_BLOCK * dk)
    return jax.nn.gelu(blk @ w1 + b1) @ w2 + b2


def _compressed_attention(q, kc, vc):
    S = q.shape[1]
    n_cmp = kc.shape[1]
    t = jnp.arange(S)
    blk_end = jnp.arange(n_cmp) * CMP_STRIDE + CMP_BLOCK - 1
    mask = blk_end[None, :] <= t[:, None]
    s = jnp.einsum('bsghd,bngd->bghsn', q, kc).astype(jnp.float32) * ATTN_SCALE
    pr = jax.nn.softmax(jnp.where(mask, s, NEG_INF), axis=-1)
    pr = pr * jnp.any(mask, axis=-1)[:, None].astype(jnp.float32)
    o = jnp.einsum('bghsn,bngd->bsghd', pr.astype(vc.dtype), vc)
    return o, pr


def _select_blocks(p_cmp, S):
    n_cmp = p_cmp.shape[-1]
    n_sel = S // SEL_BLOCK
    c_start = np.arange(n_cmp) * CMP_STRIDE
    c_end = c_start + CMP_BLOCK - 1
    s_start = np.arange(n_sel) * SEL_BLOCK
    s_end = s_start + SEL_BLOCK - 1
    overlap = ((c_start[:, None] <= s_end[None, :]) & (c_end[:, None] >= s_start[None, :])).astype(np.float32)
    imp = jnp.einsum('bghsn,nm->bgsm', p_cmp, jnp.asarray(overlap))
    t = jnp.arange(S)
    j = jnp.arange(n_sel)
    cur = t // SEL_BLOCK
    valid = s_start[None, :] <= t[:, None]
    forced = (j[None, :] == 0) | (j[None, :] == cur[:, None]) | (j[None, :] == cur[:, None] - 1)
    score = jnp.where(valid, imp + FORCE_BONUS * forced.astype(jnp.float32), -FORCE_BONUS)
    _, sel_idx = lax.top_k(score, min(SEL_TOPK, n_sel))
    return sel_idx


def _selected_attention(q, k, v, sel_idx):
    B, S, G, hpg, dk = q.shape
    n_sel = S // SEL_BLOCK
    nk = sel_idx.shape[-1]
    kb = k.reshape(B, n_sel, SEL_BLOCK, G, dk).transpose(0, 3, 1, 2, 4)
    vb = v.reshape(B, n_sel, SEL_BLOCK, G, dk).transpose(0, 3, 1, 2, 4)
    nc = S // SEL_Q_CHUNK
    qc = q.reshape(B, nc, SEL_Q_CHUNK, G, hpg, dk).swapaxes(0, 1)
    ic = sel_idx.reshape(B, G, nc, SEL_Q_CHUNK, nk).transpose(2, 0, 1, 3, 4)
    tc = jnp.arange(S).reshape(nc, SEL_Q_CHUNK)
    bi = jnp.arange(B)[:, None, None, None]
    gi = jnp.arange(G)[None, :, None, None]

    def chunk(args):
        q_c, i_c, t_c = args
        kg = kb[bi, gi, i_c]
        vg = vb[bi, gi, i_c].reshape(B, G, SEL_Q_CHUNK, nk * SEL_BLOCK, dk)
        kpos = i_c[..., None] * SEL_BLOCK + jnp.arange(SEL_BLOCK)
        mask = (kpos <= t_c[None, None, :, None, None]).reshape(B, G, 1, SEL_Q_CHUNK, nk * SEL_BLOCK)
        s = jnp.einsum('bqghd,bgqnld->bghqnl', q_c, kg).astype(jnp.float32) * ATTN_SCALE
        s = s.reshape(B, G, hpg, SEL_Q_CHUNK, nk * SEL_BLOCK)
        pr = jax.nn.softmax(jnp.where(mask, s, NEG_INF), axis=-1)
        return jnp.einsum('bghqm,bgqmd->bqghd', pr.astype(vg.dtype), vg)

    out = lax.map(chunk, (qc, ic, tc))
    return out.swapaxes(0, 1).reshape(B, S, G, hpg, dk)


def _window_attention(q, k, v):
    B, S, G, hpg, dk = q.shape
    nb = S // WIN_Q_BLOCK
    band = WINDOW + WIN_Q_BLOCK
    kp = jnp.pad(k, ((0, 0), (WINDOW, 0), (0, 0), (0, 0)))
    vp = jnp.pad(v, ((0, 0), (WINDOW, 0), (0, 0), (0, 0)))
    qb = q.reshape(B, nb, WIN_Q_BLOCK, G, hpg, dk).swapaxes(0, 1)

    def block(args):
        q_b, b = args
        start = b * WIN_Q_BLOCK
        kk = lax.dynamic_slice_in_dim(kp, start, band, axis=1)
        vv = lax.dynamic_slice_in_dim(vp, start, band, axis=1)
        t = start + jnp.arange(WIN_Q_BLOCK)
        kpos = start - WINDOW + jnp.arange(band)
        d = t[:, None] - kpos[None, :]
        mask = (d >= 0) & (d < WINDOW) & (kpos[None, :] >= 0)
        s = jnp.einsum('bqghd,bkgd->bghqk', q_b, kk).astype(jnp.float32) * ATTN_SCALE
        pr = jax.nn.softmax(jnp.where(mask, s, NEG_INF), axis=-1)
        return jnp.einsum('bghqk,bkgd->bqghd', pr.astype(vv.dtype), vv)

    out = lax.map(block, (qb, jnp.arange(nb)))
    return out.swapaxes(0, 1).reshape(B, S, G, hpg, dk)


def _token_mixer(x, w_in, cmp_pe, cmp_w1, cmp_b1, cmp_w2, cmp_b2, conv_w, w_nsa_out, w_conv_out, w_o):
    B, S, _ = x.shape
    G, hpg, dk = NSA_KV_GROUPS, NSA_HPG, NSA_HEAD_DIM
    sizes = [Q_DIM] + [KV_DIM] * 6 + [NSA_GATE_DIM] + [CONV_DIM] * 3 + [D_MODEL] * 2
    z = x @ w_in
    (q, k_cmp, v_cmp, k_slc, v_slc, k_win, v_win, g_nsa,
     conv_b, conv_c, conv_h, g_m_nsa, g_m_conv) = jnp.split(z, np.cumsum(sizes)[:-1].tolist(), axis=-1)
    q = q.reshape(B, S, G, hpg, dk)
    kc = _compress(k_cmp.reshape(B, S, G, dk), cmp_pe[0], cmp_w1[0], cmp_b1[0], cmp_w2[0], cmp_b2[0])
    vc = _compress(v_cmp.reshape(B, S, G, dk), cmp_pe[1], cmp_w1[1], cmp_b1[1], cmp_w2[1], cmp_b2[1])
    o_cmp, p_cmp = _compressed_attention(q, kc, vc)
    sel_idx = _select_blocks(p_cmp, S)
    o_slc = _selected_attention(q, k_slc.reshape(B, S, G, dk), v_slc.reshape(B, S, G, dk), sel_idx)
    o_win = _window_attention(q, k_win.reshape(B, S, G, dk), v_win.reshape(B, S, G, dk))
    g = jax.nn.sigmoid(g_nsa).reshape(B, S, G, hpg, NSA_BRANCHES, 1)
    o = g[..., 0, :] * o_cmp + g[..., 1, :] * o_slc + g[..., 2, :] * o_win
    y_nsa = o.reshape(B, S, Q_DIM) @ w_nsa_out
    u = conv_c * conv_h
    uc = lax.conv_general_dilated(u, conv_w[:, None, :], window_strides=(1,),
                                  padding=[(CONV_WIDTH - 1, 0)],
                                  dimension_numbers=('NWC', 'WIO', 'NWC'),
                                  feature_group_count=CONV_DIM)
    y_conv = (conv_b * uc) @ w_conv_out
    merged = jax.nn.sigmoid(g_m_nsa) * y_nsa + jax.nn.sigmoid(g_m_conv) * y_conv
    return merged @ w_o


def _hier_moe(x, rg_w, rg_b, re_w, re_b, w_gate, w_up, w_down):
    B, S, D = x.shape
    xt = x.reshape(-1, D)
    T = xt.shape[0]
    grp_p = jax.nn.softmax((xt @ rg_w + rg_b).astype(jnp.float32), axis=-1)
    grp_top, grp_idx = lax.top_k(grp_p, 1)
    exp_logits = (xt @ re_w + re_b).astype(jnp.float32).reshape(T, N_GROUPS, EXPERTS_PER_GROUP)
    chosen = jnp.take_along_axis(exp_logits, grp_idx[:, :, None], axis=1)[:, 0]
    top_v, top_i = lax.top_k(jax.nn.softmax(chosen, axis=-1), TOPK_EXPERTS)
    top_v = top_v / jnp.sum(top_v, axis=-1, keepdims=True)
    w_grp = jnp.einsum('tk,tke->te', top_v, jax.nn.one_hot(top_i, EXPERTS_PER_GROUP, dtype=jnp.float32))
    comb = (jax.nn.one_hot(grp_idx[:, 0], N_GROUPS, dtype=jnp.float32)[:, :, None]
            * w_grp[:, None, :] * grp_top[:, :, None]).reshape(T, N_EXPERTS).astype(x.dtype)
    y = jnp.zeros_like(xt)
    for e in range(N_EXPERTS):
        h = jax.nn.silu(xt @ w_gate[e]) * (xt @ w_up[e])
        y = y + comb[:, e:e + 1] * (h @ w_down[e])
    return y.reshape(B, S, D)


def setup_inputs(seed: int = 0) -> dict:
    key = jax.random.key(seed)
    ks = jax.random.split(key, 28)
    f = jnp.float32
    L = DEPTH

    def nrm(k, shape, scale):
        return jax.random.normal(k, shape, f) * scale

    return {
        "x": nrm(ks[0], (BATCH, SEQ, D_MODEL), 1.0),
        "p": nrm(ks[1], (DEPTH, BATCH, SEQ, PLE_DIM), 1.0),
        "w_in": nrm(ks[2], (L, D_MODEL, IN_PROJ_DIM), D_MODEL ** -0.5),
        "cmp_pe": nrm(ks[3], (L, 2, CMP_BLOCK, NSA_HEAD_DIM), 0.1),
        "cmp_w1": nrm(ks[4], (L, 2, CMP_BLOCK * NSA_HEAD_DIM, CMP_HIDDEN), (CMP_BLOCK * NSA_HEAD_DIM) ** -0.5),
        "cmp_b1": nrm(ks[5], (L, 2, CMP_HIDDEN), 0.01),
        "cmp_w2": nrm(ks[6], (L, 2, CMP_HIDDEN, NSA_HEAD_DIM), CMP_HIDDEN ** -0.5),
        "cmp_b2": nrm(ks[7], (L, 2, NSA_HEAD_DIM), 0.01),
        "conv_w": nrm(ks[8], (L, CONV_WIDTH, CONV_DIM), CONV_WIDTH ** -0.5),
        "w_nsa_out": nrm(ks[9], (L, Q_DIM, D_MODEL), Q_DIM ** -0.5),
        "w_conv_out": nrm(ks[10], (L, CONV_DIM, D_MODEL), CONV_DIM ** -0.5),
        "w_o": nrm(ks[11], (L, D_MODEL, D_MODEL), D_MODEL ** -0.5 * DEEPNORM_BETA),
        "ln1_g": 1.0 + nrm(ks[12], (L, D_MODEL), 0.02),
        "ln1_b": nrm(ks[13], (L, D_MODEL), 0.02),
        "router_group_w": nrm(ks[14], (L, D_MODEL, N_GROUPS), D_MODEL ** -0.5),
        "router_group_b": nrm(ks[15], (L, N_GROUPS), 0.01),
        "router_expert_w": nrm(ks[16], (L, D_MODEL, N_EXPERTS), D_MODEL ** -0.5),
        "router_expert_b": nrm(ks[17], (L, N_EXPERTS), 0.01),
        "expert_w_gate": nrm(ks[18], (L, N_EXPERTS, D_MODEL, EXPERT_HIDDEN), D_MODEL ** -0.5),
        "expert_w_up": nrm(ks[19], (L, N_EXPERTS, D_MODEL, EXPERT_HIDDEN), D_MODEL ** -0.5),
        "expert_w_down": nrm(ks[20], (L, N_EXPERTS, EXPERT_HIDDEN, D_MODEL), EXPERT_HIDDEN ** -0.5 * DEEPNORM_BETA),
        "ple_proj": nrm(ks[21], (L, PLE_DIM, D_MODEL), PLE_DIM ** -0.5),
        "ple_gate_w": nrm(ks[22], (L, D_MODEL, D_MODEL), D_MODEL ** -0.5),
        "ple_gate_b": nrm(ks[23], (L, D_MODEL), 0.01),
        "ln2_g": 1.0 + nrm(ks[24], (L, D_MODEL), 0.02),
        "ln2_b": nrm(ks[25], (L, D_MODEL), 0.02),
    }


def reference(x, p, w_in, cmp_pe, cmp_w1, cmp_b1, cmp_w2, cmp_b2, conv_w, w_nsa_out, w_conv_out, w_o,
              ln1_g, ln1_b, router_group_w, router_group_b, router_expert_w, router_expert_b,
              expert_w_gate, expert_w_up, expert_w_down, ple_proj, ple_gate_w, ple_gate_b, ln2_g, ln2_b):
    for i in range(DEPTH):
        mix = _token_mixer(x, w_in[i], cmp_pe[i], cmp_w1[i], cmp_b1[i], cmp_w2[i], cmp_b2[i],
                           conv_w[i], w_nsa_out[i], w_conv_out[i], w_o[i])
        x = _layer_norm(DEEPNORM_ALPHA * x + mix, ln1_g[i], ln1_b[i])
        ffn = _hier_moe(x, router_group_w[i], router_group_b[i], router_expert_w[i], router_expert_b[i],
                        expert_w_gate[i], expert_w_up[i], expert_w_down[i])
        ple = jax.nn.sigmoid(x @ ple_gate_w[i] + ple_gate_b[i]) * (p[i] @ ple_proj[i])
        x = _layer_norm(DEEPNORM_ALPHA * x + ffn + ple, ln2_g[i], ln2_b[i])
    return x
```

```python
from contextlib import ExitStack
import numpy as np
import concourse.bass as bass
import concourse.mybir as mybir
from concourse.bass_utils import run_bass_kernel_spmd

F32 = mybir.dt.float32
BF16 = mybir.dt.bfloat16
AF = mybir.ActivationFunctionType
ALU = mybir.AluOpType
AX = mybir.AxisListType

NCORES = 8
S = 2048
D = 1024
NT = 16
ALPHA = 2.0 ** 0.25
EPS = 1e-5
NEG = -30000.0
PAGE = 2048

CE = ("pe", "act", "dve", "pool")
ALLENG = ("pe", "act", "dve", "pool", "sp")


class Buf:
    __slots__ = ("name", "w", "r")

    def __init__(self, name):
        self.name = name
        self.w = None
        self.r = {}


class Sched:
    def __init__(self, nc, es):
        self.nc = nc
        self.es = es
        self.prog = {e: [] for e in ALLENG}
        self.sems = {}
        for e in CE:
            self.sems[e] = es.enter_context(nc.semaphore("c_" + e))
        self.cnt = {e: 0 for e in CE}
        self.seen = {e: {} for e in ALLENG}
        self.clock = {e: [None] for e in CE}
        self.dma_cnt = {}

    def buf(self, name="b"):
        return Buf(name)

    def _need(self, eng, reads, writes):
        need = {}

        def add(k, v):
            if need.get(k, 0) < v:
                need[k] = v
        for b in reads:
            if b.w is not None:
                add(*b.w)
        for b in writes:
            if b.w is not None:
                add(*b.w)
            for k, v in b.r.items():
                add(k, v)
        seen = self.seen[eng]
        for k, v in need.items():
            if k == eng and eng == "pe":
                continue
            if seen.get(k, 0) >= v:
                continue
            self.prog[eng].append(("wait", k, v))
            seen[k] = v
            if k in CE:
                snap = self.clock[k][v]
                if snap:
                    for k2, v2 in snap.items():
                        if seen.get(k2, 0) < v2:
                            seen[k2] = v2

    def _commit(self, ev, reads, writes):
        k, v = ev
        for b in reads:
            if b.r.get(k, 0) < v:
                b.r[k] = v
        for b in writes:
            b.w = ev
            b.r = {}

    def op(self, eng, fn, reads=(), writes=()):
        self._need(eng, reads, writes)
        self.cnt[eng] += 1
        v = self.cnt[eng]
        self.prog[eng].append(("op", fn, eng))
        self.clock[eng].append({k: vv for k, vv in self.seen[eng].items() if k in CE})
        self._commit((eng, v), reads, writes)

    def dma(self, eng, fn, key, reads=(), writes=()):
        self._need(eng, reads, writes)
        if key not in self.sems:
            self.sems[key] = self.es.enter_context(self.nc.semaphore("d_" + key))
            self.dma_cnt[key] = 0
        self.dma_cnt[key] += 16
        v = self.dma_cnt[key]
        self.prog[eng].append(("dma", fn, key))
        self._commit((key, v), reads, writes)

    def barrier(self):
        evs = [(e, self.cnt[e]) for e in CE if self.cnt[e] > 0]
        evs += [(k, v) for k, v in self.dma_cnt.items() if v > 0]
        for eng in ALLENG:
            for k, v in evs:
                if self.seen[eng].get(k, 0) < v:
                    self.prog[eng].append(("wait", k, v))
                    self.seen[eng][k] = v

    def emit(self):
        nc, sems, prog = self.nc, self.sems, self.prog
        with nc.Block() as block:
            def run(e, lst):
                for it in lst:
                    if it[0] == "wait":
                        e.wait_ge(sems[it[1]], it[2])
                    elif it[0] == "op":
                        it[1](e).then_inc(sems[it[2]], 1)
                    else:
                        it[1](e).then_inc(sems[it[2]], 16)

            @block.tensor
            def _(e):
                run(e, prog["pe"])

            @block.scalar
            def _(e):
                run(e, prog["act"])

            @block.vector
            def _(e):
                run(e, prog["dve"])

            @block.gpsimd
            def _(e):
                run(e, prog["pool"])

            @block.sync
            def _(e):
                run(e, prog["sp"])


class Rot:
    def __init__(self, items):
        self.items = list(items)
        self.i = 0

    def next(self):
        it = self.items[self.i % len(self.items)]
        self.i += 1
        return it


C_Q, C_KC, C_VC, C_KS, C_VS, C_KW, C_VW, C_G = 0, 512, 640, 768, 896, 1024, 1152, 1280
C_CB, C_CC, C_CH, C_G1, C_G2 = 1304, 1816, 2328, 2840, 3864
NA = 1304


def build(NSEQ, stop_after=None, dbg=None):
    nc = bass.Bass("TRN2", target_bir_lowering=False)
    es = ExitStack()

    def din(name, shape):
        return nc.dram_tensor(name, list(shape), F32, kind="ExternalInput").ap()

    xT_d = din("xT", [NSEQ, D, S])
    x_d = din("x", [NSEQ, S, D])
    pT_d = din("pT", [NSEQ, 256, S])
    w_in_d = din("w_in", [D, 4888])
    peT_d = din("peT", [2, 64, 32])
    w1_d = din("cmp_w1", [2, 2048, 128])
    b1_d = din("cmp_b1", [128, 2])
    w2_d = din("cmp_w2", [2, 128, 64])
    b2k_d = din("cmp_b2k", [64, 1])
    b2v_d = din("cmp_b2v", [1, 64])
    convw_d = din("conv_wT", [4, 128, 3])
    wnsa_d = din("w_nsa_out", [512, D])
    wcv_d = din("w_conv_out", [512, D])
    wo_d = din("w_o", [D, D])
    ln1g_d = din("ln1_g", [1, D])
    ln1b_d = din("ln1_b", [1, D])
    ln2g_d = din("ln2_g", [1, D])
    ln2b_d = din("ln2_b", [1, D])
    wr_d = din("w_router", [D, 36])
    br_d = din("b_router", [1, 36])
    wg_d = din("expert_w_gate", [32, D, 256])
    wu_d = din("expert_w_up", [32, D, 256])
    wd_d = din("expert_w_down", [32, 256, D])
    pproj_d = din("ple_proj", [256, D])
    pgw_d = din("ple_gate_w", [D, D])
    pgb_d = din("ple_gate_b", [1, D])
    c_ident = din("c_ident", [128, 128])
    c_tri = din("c_tri", [128, 512])
    c_anti = din("c_anti", [128, 512])
    c_cmpmask = din("c_cmpmask", [128, S])
    c_selconst = din("c_selconst", [128, NT * 32])
    c_overlap = din("c_overlap", [128, 32])
    c_eexp = din("c_eexp", [32, S])
    y_d = nc.dram_tensor("y", [NSEQ, S, D], F32, kind="ExternalOutput").ap()
    dbg_d = {}
    if dbg:
        for name, shape in dbg.items():
            dbg_d[name] = nc.dram_tensor("dbg_" + name, list(shape), F32, kind="ExternalOutput").ap()

    sc = Sched(nc, es)

    def sb(name, shape, dt):
        return es.enter_context(nc.sbuf_tensor(name, list(shape), dt))

    NPAGE = 38
    arena = sb("arena", [128, NPAGE * PAGE], BF16)

    def pages(p0, n):
        return arena[:, p0 * PAGE:(p0 + n) * PAGE]

    ident_bf = sb("ident_bf", [128, 128], BF16)
    ident_f = sb("ident_f", [128, 128], F32)
    tri_bf = sb("tri_bf", [128, 512], BF16)
    anti_bf = sb("anti_bf", [128, 512], BF16)
    cmpmask_bf = sb("cmpmask_bf", [128, S], BF16)
    selconst = sb("selconst", [128, NT, 32], F32)
    w2_bf = sb("w2_bf", [128, 2, 64], BF16)
    b1c = sb("b1c", [128, 2], F32)
    const1 = sb("const1", [128, 2], F32)
    b2k = sb("b2k", [64, 1], F32)
    b2v_bf = sb("b2v_bf", [1, 64], BF16)
    ones_bf = sb("ones_bf", [1, 128], BF16)
    peT_bf = sb("peT_bf", [64, 2, 32], BF16)
    convw = sb("convw", [128, 4, 3], F32)
    wr_f = sb("wr_f", [128, 8, 36], F32)
    wr_hi = sb("wr_hi", [128, 8, 36], BF16)
    wr_lo = sb("wr_lo", [128, 8, 36], BF16)
    br_b = sb("br_b", [128, 36], F32)
    pgb_bf = sb("pgb_bf", [1, D], BF16)
    vcaug = sb("vcaug", [128, 2, 97], BF16)
    kcT = sb("kcT", [64, 2, 128], BF16)
    negm = sb("negm", [128, 2, 96], BF16)
    PT = [sb(f"PT{i}", [128, 512], BF16) for i in range(4)]
    tmp = sb("tmp", [128, 12288], BF16)

    def tmpf(off, n):
        return tmp[:, 2 * off:2 * (off + n)].bitcast(F32)

    ps = [es.enter_context(nc.psum_tensor(f"ps{i}", [128, 512], F32)) for i in range(8)]
    psb = [sc.buf(f"ps{i}") for i in range(8)]

    B_const = sc.buf("const")
    cq = Rot(["pool"])

    def pdma(out, in_, key, reads=(), writes=(), maxlast=None):
        if maxlast:
            sc.dma("pool", lambda e: e.dma_start(out=out, in_=in_, max_dma_last_dim=maxlast), key, reads, writes)
        else:
            sc.dma("pool", lambda e: e.dma_start(out=out, in_=in_), key, reads, writes)

    def sdma(out, in_, key, reads=(), writes=()):
        sc.dma("sp", lambda e: e.dma_start(out=out, in_=in_), key, reads, writes)

    def mm(specs):
        def fn(e):
            last = None
            for (o, l, r, st, sp) in specs:
                last = e.matmul(o, lhsT=l, rhs=r, start=st, stop=sp)
            return last
        return fn

    def PE(specs, reads, writes):
        sc.op("pe", mm(specs), reads, writes)

    def ACT(out, in_, func, reads, writes, **kw):
        sc.op("act", lambda e: e.activation(out=out, in_=in_, func=func, **kw), reads, writes)

    def DVE(fn, reads, writes):
        sc.op("dve", fn, reads, writes)

    def POOL(fn, reads, writes):
        sc.op("pool", fn, reads, writes)

    def dump(name, ap_sb, rd, idx=None):
        if name in dbg_d:
            dst = dbg_d[name] if idx is None else dbg_d[name][idx]
            sdma(dst, ap_sb, "dbg", reads=rd)

    K = "cst"
    pdma(ident_bf[:], c_ident[:, :], K, writes=[B_const])
    sdma(ident_f[:], c_ident[:, :], "cst2", writes=[B_const])
    pdma(tri_bf[:], c_tri[:, :], K, writes=[B_const])
    pdma(anti_bf[:], c_anti[:, :], K, writes=[B_const])
    pdma(cmpmask_bf[:], c_cmpmask[:, :], K, writes=[B_const])
    sdma(selconst[:].rearrange("p a b -> p (a b)"), c_selconst[:, :], "cst2", writes=[B_const])
    pdma(w2_bf[:], w2_d.rearrange("k h d -> h k d"), K, writes=[B_const])
    sdma(b1c[:], b1_d[:, :], "cst2", writes=[B_const])
    sdma(b2k[:], b2k_d[:, :], "cst2", writes=[B_const])
    pdma(b2v_bf[:], b2v_d[:, :], K, writes=[B_const])
    pdma(peT_bf[:], peT_d.rearrange("k d l -> d k l"), K, writes=[B_const])
    sdma(convw[:], convw_d.rearrange("c p k -> p c k"), "cst2", writes=[B_const])
    sdma(wr_f[:], wr_d.rearrange("(kc p) n -> p kc n", p=128), "cst2", writes=[B_const])
    sdma(br_b[:], br_d.partition_broadcast(128).rearrange("p o n -> p (o n)"), "cst2", writes=[B_const])
    pdma(pgb_bf[:], pgb_d[:, :], K, writes=[B_const])
    for g in range(2):
        pdma(vcaug[:, g, 65:97], c_overlap[:, :], K, writes=[B_const])
    POOL(lambda e: e.memset(ones_bf[:], 1.0), [], [B_const])
    POOL(lambda e: e.memset(vcaug[:, :, 64:65], 1.0), [], [B_const])
    POOL(lambda e: e.memset(negm[:], 0.0), [], [B_const])
    sc.barrier()
    DVE(lambda e: e.tensor_copy(out=wr_hi[:], in_=wr_f[:]), [B_const], [B_const])
    DVE(lambda e: e.tensor_tensor(out=wr_lo[:], in0=wr_f[:], in1=wr_hi[:], op=ALU.subtract), [B_const], [B_const])
    sc.barrier()

    for seq in range(NSEQ):
        xT = pages(0, 8).rearrange("p (k t) -> p k t", k=8)
        qT = pages(8, 8).rearrange("p (h t) -> p h t", h=8)
        kcmpT = pages(16, 2).rearrange("p (g t) -> p g t", g=2)
        vcmpT = pages(18, 2).rearrange("p (g t) -> p g t", g=2)
        kslcT = pages(20, 2).rearrange("p (g t) -> p g t", g=2)
        kwinT = pages(22, 2).rearrange("p (g t) -> p g t", g=2)
        misc = pages(24, 4)
        vslc = misc[:, 0:2080].rearrange("p (t g c) -> p t g c", t=NT, g=2)
        vwin = misc[:, 2080:4160].rearrange("p (t g c) -> p t g c", t=NT, g=2)
        gates = misc[:, 4160:4160 + 768].bitcast(F32).rearrange("p (t c) -> p t c", t=NT)
        impacc = misc[:, 4928:4928 + 2048].bitcast(F32).rearrange("p (t g m) -> p t g m", t=NT, g=2)
        o_tok = pages(28, 4).rearrange("p (t c) -> p t c", t=NT)
        wA = pages(28, 6)[:, 0:8 * NA].rearrange("p (k c) -> p k c", k=8)
        W1 = pages(34, 4).rearrange("p (k l h) -> p k l h", k=2, l=32)

        B_xT = [sc.buf() for _ in range(8)]
        B_wA = sc.buf()
        B_W1 = sc.buf()
        B_q = [[sc.buf() for _ in range(4)] for _ in range(8)]
        B_qm = [[sc.buf() for _ in range(4)] for _ in range(8)]
        B_kc = [sc.buf() for _ in range(2)]
        B_vc = [sc.buf() for _ in range(2)]
        B_ks = [[sc.buf() for _ in range(4)] for _ in range(2)]
        B_kw = [[sc.buf() for _ in range(4)] for _ in range(2)]
        B_ee = sc.buf()
        B_v = [sc.buf() for _ in range(NT)]
        B_g = [sc.buf() for _ in range(NT)]
        B_imp = [sc.buf() for _ in range(4)]
        B_o = [sc.buf() for _ in range(4)]
        B_PT = [sc.buf() for _ in range(4)]

        for kc in range(8):
            pdma(xT[:, kc, :], xT_d[seq, kc * 128:(kc + 1) * 128, :], f"xT{kc}", writes=[B_xT[kc]])
        pdma(wA, w_in_d.rearrange("(k p) c -> p k c", p=128)[:, :, 0:NA], "wA", writes=[B_wA])
        for k in range(2):
            pdma(W1[0:64, k], w1_d[k].rearrange("(l d) h -> d l h", d=64), "W1", writes=[B_W1])
        pdma(kslcT[64:96, 0, :], c_eexp[:, :], "ee", writes=[B_ee])
        pdma(kslcT[64:96, 1, :], c_eexp[:, :], "ee", writes=[B_ee])
        DVE(lambda e: e.memset(vslc[:, :, :, 64:65], 1.0), [], B_v)
        DVE(lambda e: e.memset(vwin[:, :, :, 64:65], 1.0), [], B_v)

        rot = Rot(range(8))
        evq = Rot(["act", "dve"])

        def evac(out, in_, reads, writes):
            if evq.next() == "act":
                ACT(out, in_, AF.Copy, reads, writes)
            else:
                DVE(lambda e: e.tensor_copy(out=out, in_=in_), reads, writes)

        fm = []
        for h in range(8):
            fm.append((C_Q + 64 * h, (lambda c, h=h: qT[0:64, h, c * 512:(c + 1) * 512]), (lambda c, h=h: B_q[h][c])))
        for g in range(2):
            fm.append((C_KC + 64 * g, (lambda c, g=g: kcmpT[0:64, g, c * 512:(c + 1) * 512]), (lambda c, g=g: B_kc[g])))
            fm.append((C_VC + 64 * g, (lambda c, g=g: vcmpT[0:64, g, c * 512:(c + 1) * 512]), (lambda c, g=g: B_vc[g])))
            fm.append((C_KS + 64 * g, (lambda c, g=g: kslcT[0:64, g, c * 512:(c + 1) * 512]), (lambda c, g=g: B_ks[g][c])))
            fm.append((C_KW + 64 * g, (lambda c, g=g: kwinT[0:64, g, c * 512:(c + 1) * 512]), (lambda c, g=g: B_kw[g][c])))
        for (c0, dst, dbuf) in fm:
            for c in range(4):
                b = rot.next()
                PE([(ps[b][0:64, :], wA[:, kc, c0:c0 + 64], xT[:, kc, c * 512:(c + 1) * 512], kc == 0, kc == 7)
                    for kc in range(8)], B_xT + [B_wA], [psb[b]])
                evac(dst(c), ps[b][0:64, :], [psb[b]], [dbuf(c)])
        for t in range(NT):
            b = rot.next()
            PE([(ps[b][:, 0:408], xT[:, kc, t * 128:(t + 1) * 128], wA[:, kc, C_VS:C_VS + 408], kc == 0, kc == 7)
                for kc in range(8)], B_xT + [B_wA], [psb[b]])
            ACT(vslc[:, t, :, 0:64], ps[b][:, 0:128].rearrange("p (g c) -> p g c", g=2), AF.Copy, [psb[b]], [B_v[t]])
            DVE(lambda e, b=b, t=t: e.tensor_copy(out=vwin[:, t, :, 0:64],
                                                  in_=ps[b][:, 256:384].rearrange("p (g c) -> p g c", g=2)),
                [psb[b]], [B_v[t]])
            ACT(gates[:, t, :], ps[b][:, 384:408], AF.Sigmoid, [psb[b]], [B_g[t]])
        if dbg and "qT" in dbg_d:
            for h in range(8):
                tq = tmpf(0, 2048)
                Bt = sc.buf()
                DVE(lambda e, h=h: e.tensor_copy(out=tq[0:64, :], in_=qT[0:64, h, :]), B_q[h], [Bt])
                sdma(dbg_d["qT"][h], tq[0:64, :], "dbg", reads=[Bt])
                sc.barrier()
        sc.barrier()
        if stop_after == 1:
            continue

        if seq == 0:
            for k in range(2):
                b = rot.next()
                PE([(ps[b][:, 0:1], W1[0:64, k, l, :], peT_bf[:, k, l:l + 1], l == 0, l == 31) for l in range(32)],
                   [B_W1, B_const], [psb[b]])
                DVE(lambda e, b=b, k=k: e.tensor_tensor(out=const1[:, k:k + 1], in0=ps[b][:, 0:1], in1=b1c[:, k:k + 1], op=ALU.add),
                    [psb[b], B_const], [B_const])
        B_t = [sc.buf() for _ in range(6)]
        cx = tmpf(0, 128)
        ct2 = tmpf(128, 128)
        csg = tmpf(256, 128)
        hT = tmp[:, 1024:1024 + 128]
        for k in range(2):
            src, Bsrc = (kcmpT, B_kc) if k == 0 else (vcmpT, B_vc)
            for g in range(2):
                b = rot.next()
                PE([(ps[b][:, 0:127], W1[0:64, k, l, :], src[0:64, g, l:l + 16 * 126 + 1:16], l == 0, l == 31)
                    for l in range(32)], [B_W1, Bsrc[g]], [psb[b]])
                DVE(lambda e, b=b, k=k: e.tensor_scalar(out=cx[:, 0:127], in0=ps[b][:, 0:127], scalar1=const1[:, k:k + 1],
                                                        scalar2=None, op0=ALU.add), [psb[b], B_const], [B_t[0]])
                DVE(lambda e: e.tensor_tensor(out=ct2[:, 0:127], in0=cx[:, 0:127], in1=cx[:, 0:127], op=ALU.mult),
                    [B_t[0]], [B_t[1]])
                DVE(lambda e: e.tensor_scalar(out=ct2[:, 0:127], in0=ct2[:, 0:127], scalar1=0.044715, scalar2=1.0,
                                              op0=ALU.mult, op1=ALU.add), [B_t[1]], [B_t[1]])
                DVE(lambda e: e.tensor_tensor(out=ct2[:, 0:127], in0=ct2[:, 0:127], in1=cx[:, 0:127], op=ALU.mult),
                    [B_t[0], B_t[1]], [B_t[1]])
                ACT(csg[:, 0:127], ct2[:, 0:127], AF.Sigmoid, [B_t[1]], [B_t[2]], scale=1.5957691216057308)
                DVE(lambda e: e.tensor_tensor(out=hT[:, 0:127], in0=cx[:, 0:127], in1=csg[:, 0:127], op=ALU.mult),
                    [B_t[0], B_t[2]], [B_t[3]])
                b2 = rot.next()
                if k == 0:
                    PE([(ps[b2][0:64, 0:127], w2_bf[:, 0, :], hT[:, 0:127], True, True)], [B_t[3], B_const], [psb[b2]])
                    DVE(lambda e, b2=b2, g=g: e.tensor_scalar(out=kcT[:, g, 0:127], in0=ps[b2][0:64, 0:127], scalar1=b2k[:, 0:1],
                                                              scalar2=None, op0=ALU.add), [psb[b2], B_const], [B_kc[g]])
                else:
                    PE([(ps[b2][0:127, 0:64], hT[:, 0:127], w2_bf[:, 1, :], True, False),
                        (ps[b2][0:127, 0:64], ones_bf[0:1, 0:127], b2v_bf[0:1, :], False, True)],
                       [B_t[3], B_const], [psb[b2]])
                    DVE(lambda e, b2=b2, g=g: e.tensor_copy(out=vcaug[0:127, g, 0:64], in_=ps[b2][0:127, 0:64]),
                        [psb[b2]], [B_vc[g]])
        sc.barrier()

        sbank = Rot([0, 1, 2, 3])
        abank = Rot([4, 5])
        ptrot = Rot(range(4))
        rs = tmpf(0, 4)
        sgate = tmpf(8, 4)
        otmp = tmpf(16, 256)
        itmp = tmpf(272, 128)
        B_rs, B_sg, B_ot, B_it = sc.buf(), sc.buf(), sc.buf(), sc.buf()

        def evac_attn(ab, hh, c, br, ncol, first):
            acc = ps[ab][:, :].rearrange("p (j c) -> p j c", j=4)
            g = hh // 4
            DVE(lambda e: e.tensor_scalar(out=rs[:, 0:4], in0=acc[:, :, 64:65].rearrange("p j o -> p (j o)"), scalar1=1e-30,
                                          scalar2=None, op0=ALU.add), [psb[ab]], [B_rs])
            DVE(lambda e: e.reciprocal(out=rs[:, 0:4], in_=rs[:, 0:4]), [B_rs], [B_rs])
            DVE(lambda e: e.tensor_tensor(out=sgate[:, 0:4], in0=rs[:, 0:4], in1=gates[:, 4 * c:4 * c + 4, hh * 3 + br],
                                          op=ALU.mult), [B_rs] + B_g[4 * c:4 * c + 4], [B_sg])
            dst = o_tok[:, 4 * c:4 * c + 4, hh * 64:(hh + 1) * 64]
            sgb = sgate[:, 0:4].unsqueeze(2).to_broadcast([128, 4, 64])
            if first:
                DVE(lambda e: e.tensor_tensor(out=dst, in0=acc[:, :, 0:64], in1=sgb, op=ALU.mult),
                    [psb[ab], B_sg], [B_o[c]])
            else:
                ot = otmp[:, 0:256].rearrange("p (j c) -> p j c", j=4)
                DVE(lambda e: e.tensor_tensor(out=ot, in0=acc[:, :, 0:64], in1=sgb, op=ALU.mult),
                    [psb[ab], B_sg], [B_ot])
                DVE(lambda e: e.tensor_tensor(out=dst, in0=dst, in1=ot, op=ALU.add), [B_ot, B_o[c]], [B_o[c]])
            if br == 0:
                rsb = rs[:, 0:4].unsqueeze(2).to_broadcast([128, 4, 32])
                idst = impacc[:, 4 * c:4 * c + 4, g, :]
                if hh % 4 == 0:
                    DVE(lambda e: e.tensor_tensor(out=idst, in0=acc[:, :, 65:97], in1=rsb, op=ALU.mult),
                        [psb[ab], B_rs], [B_imp[c]])
                else:
                    it = itmp[:, 0:128].rearrange("p (j c) -> p j c", j=4)
                    DVE(lambda e: e.tensor_tensor(out=it, in0=acc[:, :, 65:97], in1=rsb, op=ALU.mult),
                        [psb[ab], B_rs], [B_it])
                    DVE(lambda e: e.tensor_tensor(out=idst, in0=idst, in1=it, op=ALU.add), [B_it, B_imp[c]], [B_imp[c]])

        for hh in range(8):
            g = hh // 4
            for c in range(4):
                b = sbank.next()
                PE([(ps[b][0:127, :], kcT[:, g, 0:127], qT[0:64, hh, c * 512:(c + 1) * 512], True, False),
                    (ps[b][0:127, :], ident_bf[0:127, 0:127], cmpmask_bf[0:127, c * 512:(c + 1) * 512], False, True)],
                   [B_kc[g], B_q[hh][c], B_const], [psb[b]])
                pi = ptrot.next()
                ACT(PT[pi][0:127, :], ps[b][0:127, :], AF.Exp, [psb[b]], [B_PT[pi]], scale=0.125)
                ab = abank.next()
                PE([(ps[ab][:, j * 128:j * 128 + 97], PT[pi][0:127, j * 128:(j + 1) * 128], vcaug[0:127, g, :], True, True)
                    for j in range(4)], [B_PT[pi], B_vc[g], B_const], [psb[ab]])
                evac_attn(ab, hh, c, 0, 128, True)
        score = tmpf(512, 32)
        work = tmpf(544, 32)
        m8a = tmpf(576, 8)
        m8b = tmpf(584, 8)
        msk = tmpf(592, 32)
        B_s = [sc.buf() for _ in range(5)]
        B_negm = sc.buf()
        for c in range(4):
            for g in range(2):
                b = sbank.next()
                for j in range(4):
                    t = 4 * c + j
                    DVE(lambda e, t=t, g=g: e.tensor_tensor(out=score[:, :], in0=impacc[:, t, g, :], in1=selconst[:, t, :], op=ALU.add),
                        [B_imp[c], B_const], [B_s[0]])
                    DVE(lambda e: e.max(out=m8a[:, :], in_=score[:, :]), [B_s[0]], [B_s[1]])
                    DVE(lambda e: e.match_replace(out=work[:, :], in_to_replace=m8a[:, :], in_values=score[:, :], imm_value=-1e9),
                        [B_s[0], B_s[1]], [B_s[2]])
                    DVE(lambda e: e.max(out=m8b[:, :], in_=work[:, :]), [B_s[2]], [B_s[3]])
                    DVE(lambda e: e.tensor_scalar(out=msk[:, :], in0=score[:, :], scalar1=m8b[:, 7:8], scalar2=None, op0=ALU.is_ge),
                        [B_s[0], B_s[3]], [B_s[4]])
                    DVE(lambda e, g=g: e.tensor_scalar(out=negm[:, g, 64:96], in0=msk[:, :], scalar1=-1.0, scalar2=-NEG,
                                                       op0=ALU.add, op1=ALU.mult), [B_s[4]], [B_negm])
                    PE([(ps[b][0:96, j * 128:(j + 1) * 128], negm[:, g, :], ident_bf[:, :], True, True)],
                       [B_negm, B_const], [psb[b]])
                    if "msk" in dbg_d:
                        sdma(dbg_d["msk"][g, t], msk[:, :], "dbg", reads=[B_s[4]])
                        sc.barrier()
                DVE(lambda e, b=b, g=g, c=c: e.tensor_copy(
                    out=qT[64:96, 4 * g:4 * g + 4, c * 512:(c + 1) * 512],
                    in_=ps[b][64:96, :].unsqueeze(1).to_broadcast([32, 4, 512])),
                    [psb[b]], [B_qm[4 * g + h][c] for h in range(4)])
        sc.barrier()

        def attn_branch(br, kT, Bk, vtok, krows):
            for hh in range(8):
                g = hh // 4
                for c in range(4):
                    ab = abank.next()
                    kt0 = 0 if br == 1 else max(0, 4 * c - 4)
                    kts = list(range(kt0, 4 * c + 4))
                    for kt in kts:
                        r = kt - 4 * c
                        if r >= 0:
                            j0, j1 = r, 4
                        elif br == 1:
                            j0, j1 = 0, 4
                        else:
                            j0, j1 = 0, r + 5
                        q0, q1 = c * 512 + j0 * 128, c * 512 + j1 * 128
                        N = q1 - q0
                        b = sbank.next()
                        specs = []
                        rd = [Bk[g][kt // 4], B_q[hh][c], B_const]
                        if br == 1:
                            rd += [B_qm[hh][c], B_ee]
                        if r >= 0:
                            specs.append((ps[b][:, 0:N], ident_bf[:, :], tri_bf[:, 0:N], True, False))
                        elif br == 2:
                            specs.append((ps[b][:, 0:N], ident_bf[:, :], anti_bf[:, 512 - N:512], True, False))
                        specs.append((ps[b][:, 0:N], kT[0:krows, g, kt * 128:(kt + 1) * 128], qT[0:krows, hh, q0:q1],
                                      len(specs) == 0, True))
                        PE(specs, rd, [psb[b]])
                        pi = ptrot.next()
                        ACT(PT[pi][:, 0:N], ps[b][:, 0:N], AF.Exp, [psb[b]], [B_PT[pi]], scale=0.125)
                        pv = []
                        for j in range(j0, j1):
                            first_kt = 0 if br == 1 else max(0, 4 * c + j - 4)
                            last_kt = 4 * c + j
                            pv.append((ps[ab][:, j * 128:j * 128 + 65], PT[pi][:, (j - j0) * 128:(j - j0 + 1) * 128],
                                       vtok[:, kt, g, :], (kt == kts[0] and j == j0), kt == last_kt))
                        PE(pv, [B_PT[pi], B_v[kt]], [psb[ab]])
                    evac_attn(ab, hh, c, br, 128, False)

        attn_branch(1, kslcT, B_ks, vslc, 96)
        attn_branch(2, kwinT, B_kw, vwin, 64)
        sc.barrier()
        if "o_tok" in dbg_d:
            for t in range(NT):
                tq = tmpf(0, 512)
                DVE(lambda e, t=t: e.tensor_copy(out=tq[:, :], in_=o_tok[:, t, :]), [], [])
                sc.barrier()
                sdma(dbg_d["o_tok"][t * 128:(t + 1) * 128, :], tq[:, :], "dbg")
                sc.barrier()
        if stop_after == 2:
            continue

        oT = pages(8, 4).rearrange("p (i t) -> p i t", i=4)
        B_oT = [sc.buf() for _ in range(4)]
        for c in range(4):
            for i in range(4):
                b = rot.next()
                pb = ps[b][:, :].bitcast(BF16)
                sc.op("pe", (lambda b=b, c=c, i=i, pb=pb: (lambda e: [e.transpose(pb[:, j * 128:(j + 1) * 128], o_tok[:, 4 * c + j, i * 128:(i + 1) * 128], ident_bf[:, :]) for j in range(4)][-1]))(),
                      [B_o[c], B_const], [psb[b]])
                evac(oT[:, i, c * 512:(c + 1) * 512], pb[:, 0:512], [psb[b]], [B_oT[c]])
        sc.barrier()

        buT = pages(12, 4).rearrange("p (i t) -> p i t", i=4)
        mergedT = pages(16, 8).rearrange("p (k t) -> p k t", k=8)
        wcB = pages(24, 6).rearrange("p (k c) -> p k c", k=8)
        wgm = [pages(30, 2).rearrange("p (k c) -> p k c", k=8), pages(32, 2).rearrange("p (k c) -> p k c", k=8)]
        wnsa = pages(34, 2).rearrange("p (k c) -> p k c", k=4)
        wcv = pages(36, 2).rearrange("p (k c) -> p k c", k=4)
        B_wcB, B_wnsa, B_wcv = sc.buf(), sc.buf(), sc.buf()
        B_wgm = [sc.buf(), sc.buf()]
        B_bu = [sc.buf() for _ in range(4)]
        B_mg = [sc.buf() for _ in range(4)]
        w_in_v = w_in_d.rearrange("(k p) c -> p k c", p=128)
        pdma(wcB, w_in_v[:, :, C_CB:C_CB + 1536], "wcB", writes=[B_wcB])
        pdma(wnsa, wnsa_d.rearrange("(k p) c -> p k c", p=128), "wnsa", writes=[B_wnsa])
        pdma(wcv, wcv_d.rearrange("(k p) c -> p k c", p=128), "wcv", writes=[B_wcv])
        c_sb = tmpf(0, 512)
        u = tmpf(512, 514)
        uc = tmpf(1026, 512)
        B_c, B_u, B_uc = sc.buf(), sc.buf(), sc.buf()
        for cb in range(4):
            for c in range(4):
                bb, bc, bh = rot.next(), rot.next(), rot.next()
                for (bk, off) in ((bb, 0), (bc, 512), (bh, 1024)):
                    PE([(ps[bk][:, :], wcB[:, kc, off + cb * 128:off + (cb + 1) * 128], xT[:, kc, c * 512:(c + 1) * 512], kc == 0, kc == 7)
                        for kc in range(8)], B_xT + [B_wcB], [psb[bk]])
                if c == 0:
                    DVE(lambda e: e.memset(u[:, 0:2], 0.0), [B_u], [B_u])
                else:
                    DVE(lambda e: e.tensor_copy(out=u[:, 0:2], in_=u[:, 512:514]), [B_u], [B_u])
                ACT(c_sb[:, :], ps[bc][:, :], AF.Copy, [psb[bc]], [B_c])
                DVE(lambda e, bh=bh: e.tensor_tensor(out=u[:, 2:514], in0=c_sb[:, :], in1=ps[bh][:, :], op=ALU.mult),
                    [B_c, psb[bh]], [B_u])
                DVE(lambda e, cb=cb: e.tensor_scalar(out=uc[:, :], in0=u[:, 0:512], scalar1=convw[:, cb, 0:1], scalar2=None, op0=ALU.mult),
                    [B_u, B_const], [B_uc])
                DVE(lambda e, cb=cb: e.scalar_tensor_tensor(out=uc[:, :], in0=u[:, 1:513], scalar=convw[:, cb, 1:2], in1=uc[:, :],
                                                            op0=ALU.mult, op1=ALU.add), [B_u, B_uc], [B_uc])
                DVE(lambda e, cb=cb: e.scalar_tensor_tensor(out=uc[:, :], in0=u[:, 2:514], scalar=convw[:, cb, 2:3], in1=uc[:, :],
                                                            op0=ALU.mult, op1=ALU.add), [B_u, B_uc], [B_uc])
                DVE(lambda e, bb=bb, cb=cb, c=c: e.tensor_tensor(out=buT[:, cb, c * 512:(c + 1) * 512], in0=uc[:, :], in1=ps[bb][:, :], op=ALU.mult),
                    [B_uc, psb[bb]], [B_bu[c]])
        s1 = tmpf(0, 512)
        s2 = tmpf(512, 512)
        m1 = tmpf(1024, 512)
        m2 = tmpf(1536, 512)
        B_s1, B_s2, B_m1, B_m2 = sc.buf(), sc.buf(), sc.buf(), sc.buf()
        for dc in range(8):
            wq = wgm[dc % 2]
            pdma(wq[:, :, 0:128], w_in_v[:, :, C_G1 + dc * 128:C_G1 + (dc + 1) * 128], f"wgm{dc % 2}", writes=[B_wgm[dc % 2]])
            pdma(wq[:, :, 128:256], w_in_v[:, :, C_G2 + dc * 128:C_G2 + (dc + 1) * 128], f"wgm{dc % 2}", writes=[B_wgm[dc % 2]])
            for c in range(4):
                b1, b2, b3, b4 = rot.next(), rot.next(), rot.next(), rot.next()
                tok = slice(c * 512, (c + 1) * 512)
                PE([(ps[b1][:, :], wq[:, kc, 0:128], xT[:, kc, tok], kc == 0, kc == 7) for kc in range(8)],
                   B_xT + [B_wgm[dc % 2]], [psb[b1]])
                PE([(ps[b2][:, :], wq[:, kc, 128:256], xT[:, kc, tok], kc == 0, kc == 7) for kc in range(8)],
                   B_xT + [B_wgm[dc % 2]], [psb[b2]])
                PE([(ps[b3][:, :], wnsa[:, i, dc * 128:(dc + 1) * 128], oT[:, i, tok], i == 0, i == 3) for i in range(4)],
                   [B_wnsa, B_oT[c]], [psb[b3]])
                PE([(ps[b4][:, :], wcv[:, i, dc * 128:(dc + 1) * 128], buT[:, i, tok], i == 0, i == 3) for i in range(4)],
                   [B_wcv, B_bu[c]], [psb[b4]])
                ACT(s1[:, :], ps[b1][:, :], AF.Sigmoid, [psb[b1]], [B_s1])
                ACT(s2[:, :], ps[b2][:, :], AF.Sigmoid, [psb[b2]], [B_s2])
                DVE(lambda e, b3=b3: e.tensor_tensor(out=m1[:, :], in0=s1[:, :], in1=ps[b3][:, :], op=ALU.mult),
                    [B_s1, psb[b3]], [B_m1])
                DVE(lambda e, b4=b4: e.tensor_tensor(out=m2[:, :], in0=s2[:, :], in1=ps[b4][:, :], op=ALU.mult),
                    [B_s2, psb[b4]], [B_m2])
                DVE(lambda e, dc=dc, tok=tok: e.tensor_tensor(out=mergedT[:, dc, tok], in0=m1[:, :], in1=m2[:, :], op=ALU.add),
                    [B_m1, B_m2], [B_mg[c]])
        sc.barrier()
        if stop_after == 3:
            continue

        pT_bf = pages(13, 2).rearrange("p (k t) -> p k t", k=2)
        B_pT = sc.buf()
        pdma(pT_bf, pT_d[seq].rearrange("(k p) t -> p k t", p=128), "pT", writes=[B_pT])
        lnt = pages(9, 4).bitcast(F32).rearrange("p (a d) -> p a d", a=4)
        B_ln = sc.buf()
        for a, src in enumerate((ln1g_d, ln1b_d, ln2g_d, ln2b_d)):
            sdma(lnt[:, a, :], src.partition_broadcast(128).rearrange("p o d -> p (o d)"), "ln", writes=[B_ln])
        pproj = pages(8, 1).rearrange("p (k c) -> p k c", k=2)
        B_pproj = sc.buf()
        pdma(pproj, pproj_d.rearrange("(k p) c -> p k c", p=128), "pproj", writes=[B_pproj])
        x1T = arena[:, 15 * PAGE:16 * PAGE]
        x1T = [arena[:, 15 * PAGE:16 * PAGE].rearrange("p (k t) -> p k t", k=2),
               pages(24, 3).rearrange("p (k t) -> p k t", k=6)]

        def x1T_ap(kc, t0, t1):
            return x1T[0][:, kc, t0:t1] if kc < 2 else x1T[1][:, kc - 2, t0:t1]
        y_acc = pages(27, 8).bitcast(F32).rearrange("p (t d) -> p t d", t=8)
        comb_all = tmpf(4864, 256).rearrange("p (t e) -> p t e", t=8)
        for half in range(2):
            wo = pages(0, 4).rearrange("p (k c) -> p k c", k=8)
            pgw = pages(4, 4).rearrange("p (k c) -> p k c", k=8)
            B_wo, B_pgw = sc.buf(), sc.buf()
            pdma(wo, wo_d.rearrange("(k p) c -> p k c", p=128), "wo", writes=[B_wo])
            pdma(pgw, pgw_d.rearrange("(k p) c -> p k c", p=128), "pgw", writes=[B_pgw])
            B_x1T = [sc.buf() for _ in range(8)]
            B_y = [sc.buf() for _ in range(8)]
            B_comb = [sc.buf() for _ in range(8)]
            xt = [tmpf(0, 1024), tmpf(1024, 1024)]
            B_xt = [sc.buf(), sc.buf()]
            h1 = tmpf(2048, 1024)
            x_hi = tmp[:, 6144:7168]
            x_lo = tmp[:, 7168:8192]
            x1Tlo = tmp[:, 10240:11264].rearrange("p (k t) -> p k t", k=8)
            B_xhi, B_xlo = sc.buf(), sc.buf()
            sgt = tmpf(4096, 512)
            st6 = tmpf(4608, 16)
            mv = tmpf(4624, 4)
            rt = tmpf(4640, 96)
            B_h1, B_x1Tf, B_sgt, B_st, B_mv = sc.buf(), sc.buf(), sc.buf(), sc.buf(), sc.buf()
            B_rt = [sc.buf() for _ in range(12)]

            def layernorm(src, Bsrc, dst, Bdst, ga, ba):
                for hf in range(2):
                    DVE(lambda e, hf=hf: e.bn_stats(out=st6[:, hf * 6:(hf + 1) * 6], in_=src[:, hf * 512:(hf + 1) * 512]),
                        [Bsrc], [B_st])
                DVE(lambda e: e.bn_aggr(out=mv[:, 0:2], in_=st6[:, 0:12]), [B_st], [B_mv])
                DVE(lambda e: e.tensor_scalar(out=mv[:, 1:2], in0=mv[:, 1:2], scalar1=EPS, scalar2=None, op0=ALU.add),
                    [B_mv], [B_mv])
                ACT(mv[:, 1:2], mv[:, 1:2], AF.Sqrt, [B_mv], [B_mv])
                DVE(lambda e: e.reciprocal(out=mv[:, 1:2], in_=mv[:, 1:2]), [B_mv], [B_mv])
                DVE(lambda e: e.tensor_scalar(out=dst, in0=src, scalar1=mv[:, 0:1], scalar2=mv[:, 1:2], op0=ALU.subtract, op1=ALU.mult),
                    [Bsrc, B_mv], [Bdst])
                DVE(lambda e: e.tensor_tensor(out=dst, in0=dst, in1=lnt[:, ga, :], op=ALU.mult), [Bdst, B_ln], [Bdst])
                DVE(lambda e: e.tensor_tensor(out=dst, in0=dst, in1=lnt[:, ba, :], op=ALU.add), [Bdst, B_ln], [Bdst])

            for tl in range(8):
                t = half * 8 + tl
                tsl = slice(t * 128, (t + 1) * 128)
                xb = tl % 2
                sdma(xt[xb][:, :], x_d[seq, tsl, :], f"xt{xb}", writes=[B_xt[xb]])
                bm = (rot.next(), rot.next())
                for hf in range(2):
                    PE([(ps[bm[hf]][:, :], mergedT[:, dc, tsl], wo[:, dc, hf * 512:(hf + 1) * 512], dc == 0, dc == 7) for dc in range(8)],
                       [B_mg[t // 4], B_wo], [psb[bm[hf]]])
                    DVE(lambda e, hf=hf, b=bm[hf], xb=xb: e.scalar_tensor_tensor(
                        out=h1[:, hf * 512:(hf + 1) * 512], in0=xt[xb][:, hf * 512:(hf + 1) * 512], scalar=ALPHA,
                        in1=ps[b][:, :], op0=ALU.mult, op1=ALU.add), [B_xt[xb], psb[bm[hf]]], [B_h1])
                layernorm(h1[:, :], B_h1, h1[:, :], B_h1, 0, 1)
                if "x1" in dbg_d:
                    sdma(dbg_d["x1"][tsl, :], h1[:, :], "dbg", reads=[B_h1])
                DVE(lambda e: e.tensor_copy(out=x_hi[:, :], in_=h1[:, :]), [B_h1], [B_xhi])
                DVE(lambda e: e.tensor_tensor(out=x_lo[:, :], in0=h1[:, :], in1=x_hi[:, :], op=ALU.subtract), [B_h1, B_xhi], [B_xlo])
                bt = (rot.next(), rot.next())
                for which, (srcx, Bsrcx) in enumerate(((x_hi, B_xhi), (x_lo, B_xlo))):
                    b = bt[which]
                    pb = ps[b][:, :].bitcast(BF16)
                    sc.op("pe", (lambda pb=pb, srcx=srcx: (lambda e: [e.transpose(pb[:, j * 128:(j + 1) * 128], srcx[:, j * 128:(j + 1) * 128], ident_bf[:, :]) for j in range(8)][-1]))(),
                          [Bsrcx, B_const], [psb[b]])
                    if which == 0:
                        ACT(x1T[0][:, :, tl * 128:(tl + 1) * 128], pb[:, 0:256].rearrange("p (k t) -> p k t", k=2), AF.Copy,
                            [psb[b]], [B_x1T[tl]])
                        DVE(lambda e, pb=pb, tl=tl: e.tensor_copy(out=x1T[1][:, :, tl * 128:(tl + 1) * 128],
                                                                    in_=pb[:, 256:1024].rearrange("p (k t) -> p k t", k=6)),
                            [psb[b]], [B_x1T[tl]])
                    else:
                        ACT(x1Tlo[:, :, :], pb[:, :].rearrange("p (k t) -> p k t", k=8), AF.Copy, [psb[b]], [B_x1Tf])
                br_ = rot.next()
                specs = []
                for kc in range(8):
                    specs.append((ps[br_][:, 0:36], x1T_ap(kc, tl * 128, (tl + 1) * 128), wr_hi[:, kc, :], kc == 0, False))
                    specs.append((ps[br_][:, 0:36], x1Tlo[:, kc, :], wr_hi[:, kc, :], False, False))
                    specs.append((ps[br_][:, 0:36], x1T_ap(kc, tl * 128, (tl + 1) * 128), wr_lo[:, kc, :], False, kc == 7))
                PE(specs, [B_x1Tf, B_x1T[tl], B_const], [psb[br_]])
                lg = rt[:, 0:36]
                gmax, gsum, oh, ch, m8, wv, w8a, w8b, wg8 = (rt[:, 36:37], rt[:, 37:38], rt[:, 40:44], rt[:, 44:52], rt[:, 52:60],
                                                            rt[:, 60:62], rt[:, 64:72], rt[:, 72:80], rt[:, 80:88])
                gex = rt[:, 88:92]
                DVE(lambda e, b=br_: e.tensor_tensor(out=lg, in0=ps[b][:, 0:36], in1=br_b[:, :], op=ALU.add), [psb[br_], B_const], [B_rt[0]])
                DVE(lambda e: e.tensor_reduce(out=gmax, in_=lg[:, 0:4], axis=AX.X, op=ALU.max), [B_rt[0]], [B_rt[1]])
                DVE(lambda e: e.tensor_scalar(out=oh, in0=lg[:, 0:4], scalar1=gmax, scalar2=None, op0=ALU.is_ge), [B_rt[0], B_rt[1]], [B_rt[2]])
                DVE(lambda e: e.tensor_scalar(out=gex, in0=lg[:, 0:4], scalar1=gmax, scalar2=None, op0=ALU.subtract), [B_rt[0], B_rt[1]], [B_rt[3]])
                ACT(gex, gex, AF.Exp, [B_rt[3]], [B_rt[3]])
                DVE(lambda e: e.tensor_reduce(out=gsum, in_=gex, axis=AX.X, op=ALU.add), [B_rt[3]], [B_rt[4]])
                DVE(lambda e: e.reciprocal(out=gsum, in_=gsum), [B_rt[4]], [B_rt[4]])
                DVE(lambda e: e.tensor_scalar(out=oh, in0=oh, scalar1=gsum, scalar2=None, op0=ALU.mult), [B_rt[2], B_rt[4]], [B_rt[2]])
                ohu = rt[:, 92:96]
                DVE(lambda e: e.tensor_scalar(out=ohu, in0=lg[:, 0:4], scalar1=gmax, scalar2=None, op0=ALU.is_ge), [B_rt[0], B_rt[1]], [B_rt[5]])
                DVE(lambda e: e.tensor_scalar(out=ch, in0=lg[:, 4:12], scalar1=ohu[:, 0:1], scalar2=None, op0=ALU.mult), [B_rt[0], B_rt[5]], [B_rt[6]])
                for g in range(1, 4):
                    DVE(lambda e, g=g: e.scalar_tensor_tensor(out=ch, in0=lg[:, 4 + 8 * g:12 + 8 * g], scalar=ohu[:, g:g + 1], in1=ch,
                                                              op0=ALU.mult, op1=ALU.add), [B_rt[0], B_rt[5], B_rt[6]], [B_rt[6]])
                DVE(lambda e: e.max(out=m8, in_=ch), [B_rt[6]], [B_rt[7]])
                DVE(lambda e: e.tensor_tensor(out=wv[:, 0:1], in0=m8[:, 0:1], in1=m8[:, 1:2], op=ALU.subtract), [B_rt[7]], [B_rt[8]])
                ACT(wv[:, 0:1], wv[:, 0:1], AF.Sigmoid, [B_rt[8]], [B_rt[8]])
                DVE(lambda e: e.tensor_scalar(out=wv[:, 1:2], in0=wv[:, 0:1], scalar1=-1.0, scalar2=1.0, op0=ALU.mult, op1=ALU.add), [B_rt[8]], [B_rt[8]])
                DVE(lambda e: e.tensor_scalar(out=w8a, in0=ch, scalar1=m8[:, 0:1], scalar2=wv[:, 0:1], op0=ALU.is_equal, op1=ALU.mult),
                    [B_rt[6], B_rt[7], B_rt[8]], [B_rt[9]])
                DVE(lambda e: e.tensor_scalar(out=w8b, in0=ch, scalar1=m8[:, 1:2], scalar2=wv[:, 1:2], op0=ALU.is_equal, op1=ALU.mult),
                    [B_rt[6], B_rt[7], B_rt[8]], [B_rt[10]])
                DVE(lambda e: e.tensor_tensor(out=wg8, in0=w8a, in1=w8b, op=ALU.add), [B_rt[9], B_rt[10]], [B_rt[11]])
                DVE(lambda e, tl=tl: e.tensor_tensor(out=comb_all[:, tl, :].rearrange("p (g x) -> p g x", g=4),
                                                     in0=oh.unsqueeze(2).to_broadcast([128, 4, 8]),
                                                     in1=wg8.unsqueeze(1).to_broadcast([128, 4, 8]), op=ALU.mult),
                    [B_rt[2], B_rt[11]], [B_comb[tl]])
                if "comb" in dbg_d:
                    sdma(dbg_d["comb"][tsl, :], comb_all[:, tl, :], "dbg", reads=[B_comb[tl]])
                for hf in range(2):
                    bg, bp = rot.next(), rot.next()
                    cs = slice(hf * 512, (hf + 1) * 512)
                    PE([(ps[bg][:, :], x1T_ap(kc, tl * 128, (tl + 1) * 128), pgw[:, kc, cs], kc == 0, False) for kc in range(8)]
                       + [(ps[bg][:, :], ones_bf[0:1, :], pgb_bf[0:1, cs], False, True)],
                       [B_x1T[tl], B_pgw, B_const], [psb[bg]])
                    PE([(ps[bp][:, :], pT_bf[:, k2, tsl], pproj[:, k2, cs], k2 == 0, k2 == 1) for k2 in range(2)],
                       [B_pT, B_pproj], [psb[bp]])
                    ACT(sgt[:, :], ps[bg][:, :], AF.Sigmoid, [psb[bg]], [B_sgt])
                    DVE(lambda e, bp=bp: e.tensor_tensor(out=sgt[:, :], in0=sgt[:, :], in1=ps[bp][:, :], op=ALU.mult), [B_sgt, psb[bp]], [B_sgt])
                    DVE(lambda e, tl=tl, cs=cs: e.scalar_tensor_tensor(out=y_acc[:, tl, cs], in0=h1[:, cs], scalar=ALPHA, in1=sgt[:, :],
                                                                        op0=ALU.mult, op1=ALU.add), [B_h1, B_sgt], [B_y[tl]])
            sc.barrier()
            if stop_after == 4:
                continue
            wgu = [pages(0, 2).rearrange("p (k c) -> p k c", k=8), pages(3, 2).rearrange("p (k c) -> p k c", k=8)]
            wdn = [pages(2, 1).rearrange("p (k c) -> p k c", k=2), pages(5, 1).rearrange("p (k c) -> p k c", k=2)]
            B_wgu, B_wdn = [sc.buf(), sc.buf()], [sc.buf(), sc.buf()]
            sg_t = [tmp[:, 0:256], tmp[:, 256:512]]
            h_t = [tmp[:, 512:768], tmp[:, 768:1024]]
            hT_t = [tmp[:, 1024:1280].rearrange("p (j t) -> p j t", j=2), tmp[:, 1280:1536].rearrange("p (j t) -> p j t", j=2)]
            B_sgm, B_hm, B_hTm = [sc.buf(), sc.buf()], [sc.buf(), sc.buf()], [sc.buf(), sc.buf()]
            gub = Rot([0, 1])
            htb = Rot([2, 3])
            yb = Rot([(4, 5), (6, 7)])
            it = 0
            for ex in range(32):
                wb = ex % 2
                pdma(wgu[wb][:, :, 0:256], wg_d[ex].rearrange("(k p) c -> p k c", p=128), f"wg{wb}", writes=[B_wgu[wb]])
                pdma(wgu[wb][:, :, 256:512], wu_d[ex].rearrange("(k p) c -> p k c", p=128), f"wg{wb}", writes=[B_wgu[wb]])
                pdma(wdn[wb], wd_d[ex].rearrange("(k p) c -> p k c", p=128), f"wd{wb}", writes=[B_wdn[wb]])
                for tl in range(8):
                    r2 = it % 2
                    it += 1
                    bgu = gub.next()
                    PE([(ps[bgu][:, :], x1T_ap(kc, tl * 128, (tl + 1) * 128), wgu[wb][:, kc, :], kc == 0, kc == 7) for kc in range(8)],
                       [B_x1T[tl], B_wgu[wb]], [psb[bgu]])
                    ACT(sg_t[r2][:, :], ps[bgu][:, 0:256], AF.Silu, [psb[bgu]], [B_sgm[r2]])
                    DVE(lambda e, bgu=bgu, r2=r2, tl=tl, ex=ex: e.scalar_tensor_tensor(
                        out=h_t[r2][:, :], in0=ps[bgu][:, 256:512], scalar=comb_all[:, tl, ex:ex + 1], in1=sg_t[r2][:, :],
                        op0=ALU.mult, op1=ALU.mult), [psb[bgu], B_comb[tl], B_sgm[r2]], [B_hm[r2]])
                    bh_ = htb.next()
                    pb = ps[bh_][:, :].bitcast(BF16)
                    sc.op("pe", (lambda r2=r2, pb=pb: (lambda e: [e.transpose(pb[:, j * 128:(j + 1) * 128], h_t[r2][:, j * 128:(j + 1) * 128], ident_bf[:, :]) for j in range(2)][-1]))(),
                          [B_hm[r2], B_const], [psb[bh_]])
                    ACT(hT_t[r2], pb[:, 0:256].rearrange("p (j t) -> p j t", j=2), AF.Copy, [psb[bh_]], [B_hTm[r2]])
                    by = yb.next()
                    for hf in range(2):
                        PE([(ps[by[hf]][:, :], hT_t[r2][:, j, :], wdn[wb][:, j, hf * 512:(hf + 1) * 512], j == 0, j == 1) for j in range(2)],
                           [B_hTm[r2], B_wdn[wb]], [psb[by[hf]]])
                        DVE(lambda e, tl=tl, hf=hf, b=by[hf]: e.tensor_tensor(out=y_acc[:, tl, hf * 512:(hf + 1) * 512],
                                                                               in0=y_acc[:, tl, hf * 512:(hf + 1) * 512],
                                                                               in1=ps[b][:, :], op=ALU.add),
                            [psb[by[hf]], B_y[tl]], [B_y[tl]])
            if stop_after == 5:
                sc.barrier()
                continue
            for tl in range(8):
                t = half * 8 + tl
                layernorm(y_acc[:, tl, :], B_y[tl], y_acc[:, tl, :], B_y[tl], 2, 3)
                sdma(y_d[seq, t * 128:(t + 1) * 128, :], y_acc[:, tl, :], f"yo{tl % 2}", reads=[B_y[tl]])
            sc.barrier()

    sc.barrier()
    sc.emit()
    es.close()
    return nc


def _consts():
    c = {}
    c["c_ident"] = np.eye(128, dtype=np.float32)
    a = np.arange(128)
    tri = np.zeros((128, 512), np.float32)
    tri[:, 0:128] = np.where(a[:, None] <= a[None, :], 0.0, NEG)
    c["c_tri"] = tri
    anti = np.zeros((128, 512), np.float32)
    anti[:, 384:512] = np.where(a[:, None] > a[None, :], 0.0, NEG)
    c["c_anti"] = anti
    n = np.arange(128)
    t = np.arange(S)
    c["c_cmpmask"] = np.where(n[:, None] * 16 + 31 <= t[None, :], 0.0, NEG).astype(np.float32)
    tt = (np.arange(NT)[None, :, None] * 128 + np.arange(128)[:, None, None])
    j = np.arange(32)[None, None, :]
    cur = tt // 64
    valid = j * 64 <= tt
    forced = (j == 0) | (j == cur) | (j == cur - 1)
    c["c_selconst"] = np.where(valid, 1e4 * forced, -1e4).astype(np.float32).reshape(128, NT * 32)
    cs = np.arange(128) * 16
    ce = cs + 31
    ss = np.arange(32) * 64
    se = ss + 63
    ov = ((cs[:, None] <= se[None, :]) & (ce[:, None] >= ss[None, :])).astype(np.float32)
    ov[127] = 0.0
    c["c_overlap"] = ov
    c["c_eexp"] = (np.arange(S)[None, :] // 64 == np.arange(32)[:, None]).astype(np.float32)
    return c


def _weights(inp):
    w = {}
    f = lambda a: np.ascontiguousarray(a, dtype=np.float32)
    w["w_in"] = f(inp["w_in"][0])
    w["peT"] = f(np.transpose(inp["cmp_pe"][0], (0, 2, 1)))
    w["cmp_w1"] = f(inp["cmp_w1"][0])
    w["cmp_b1"] = f(np.transpose(inp["cmp_b1"][0], (1, 0)))
    w["cmp_w2"] = f(inp["cmp_w2"][0])
    w["cmp_b2k"] = f(inp["cmp_b2"][0, 0].reshape(64, 1))
    w["cmp_b2v"] = f(inp["cmp_b2"][0, 1].reshape(1, 64))
    w["conv_wT"] = f(np.transpose(inp["conv_w"][0], (1, 0)).reshape(4, 128, 3))
    w["w_nsa_out"] = f(inp["w_nsa_out"][0])
    w["w_conv_out"] = f(inp["w_conv_out"][0])
    w["w_o"] = f(inp["w_o"][0])
    for k in ("ln1_g", "ln1_b", "ln2_g", "ln2_b", "ple_gate_b"):
        w[k] = f(inp[k][0].reshape(1, D))
    w["w_router"] = f(np.concatenate([inp["router_group_w"][0], inp["router_expert_w"][0]], axis=1))
    w["b_router"] = f(np.concatenate([inp["router_group_b"][0], inp["router_expert_b"][0]], axis=0).reshape(1, 36))
    w["expert_w_gate"] = f(inp["expert_w_gate"][0])
    w["expert_w_up"] = f(inp["expert_w_up"][0])
    w["expert_w_down"] = f(inp["expert_w_down"][0])
    w["ple_proj"] = f(inp["ple_proj"][0])
    w["ple_gate_w"] = f(inp["ple_gate_w"][0])
    return w


def kernel(**inputs):
    inp = {k: np.asarray(v) for k, v in inputs.items()}
    x = inp["x"]
    p = inp["p"][0]
    B = x.shape[0]
    nseq = B // NCORES
    shared = _weights(inp)
    shared.update(_consts())
    in_maps = []
    for c in range(NCORES):
        xs = x[c * nseq:(c + 1) * nseq]
        m = dict(shared)
        m["x"] = np.ascontiguousarray(xs)
        m["xT"] = np.ascontiguousarray(np.transpose(xs, (0, 2, 1)))
        m["pT"] = np.ascontiguousarray(np.transpose(p[c * nseq:(c + 1) * nseq], (0, 2, 1)))
        in_maps.append(m)
    nc = build(nseq)
    res = run_bass_kernel_spmd(nc, in_maps, core_ids=list(range(NCORES)))
    out = np.concatenate([np.asarray(r["y"]) for r in res.results], axis=0)
    return out.astype(np.float32, copy=False)
```

```python
from contextlib import ExitStack
import numpy as np
import concourse.bass as bass
import concourse.mybir as mybir
from concourse.bass_utils import run_bass_kernel_spmd

F32 = mybir.dt.float32
BF16 = mybir.dt.bfloat16
AF = mybir.ActivationFunctionType
ALU = mybir.AluOpType
AX = mybir.AxisListType

NCORES = 8
S = 2048
D = 1024
NT = 16
ALPHA = 2.0 ** 0.25
EPS = 1e-5
NEG = -30000.0
PAGE = 2048

CE = ("pe", "act", "dve", "pool")
ALLENG = ("pe", "act", "dve", "pool", "sp")


class Buf:
    __slots__ = ("name", "w", "r")

    def __init__(self, name):
        self.name = name
        self.w = None
        self.r = {}


class Sched:
    def __init__(self, nc, es):
        self.nc = nc
        self.es = es
        self.prog = {e: [] for e in ALLENG}
        self.sems = {}
        for e in CE:
            self.sems[e] = es.enter_context(nc.semaphore("c_" + e))
        self.cnt = {e: 0 for e in CE}
        self.seen = {e: {} for e in ALLENG}
        self.clock = {e: [None] for e in CE}
        self.dma_cnt = {}

    def buf(self, name="b"):
        return Buf(name)

    def _need(self, eng, reads, writes):
        need = {}

        def add(k, v):
            if need.get(k, 0) < v:
                need[k] = v
        for b in reads:
            if b.w is not None:
                add(*b.w)
        for b in writes:
            if b.w is not None:
                add(*b.w)
            for k, v in b.r.items():
                add(k, v)
        seen = self.seen[eng]
        for k, v in need.items():
            if k == eng and eng == "pe":
                continue
            if seen.get(k, 0) >= v:
                continue
            self.prog[eng].append(("wait", k, v))
            seen[k] = v
            if k in CE:
                snap = self.clock[k][v]
                if snap:
                    for k2, v2 in snap.items():
                        if seen.get(k2, 0) < v2:
                            seen[k2] = v2

    def _commit(self, ev, reads, writes):
        k, v = ev
        for b in reads:
            if b.r.get(k, 0) < v:
                b.r[k] = v
        for b in writes:
            b.w = ev
            b.r = {}

    def op(self, eng, fn, reads=(), writes=()):
        self._need(eng, reads, writes)
        self.cnt[eng] += 1
        v = self.cnt[eng]
        self.prog[eng].append(("op", fn, eng))
        self.clock[eng].append({k: vv for k, vv in self.seen[eng].items() if k in CE})
        self._commit((eng, v), reads, writes)

    def dma(self, eng, fn, key, reads=(), writes=()):
        self._need(eng, reads, writes)
        if key not in self.sems:
            self.sems[key] = self.es.enter_context(self.nc.semaphore("d_" + key))
            self.dma_cnt[key] = 0
        self.dma_cnt[key] += 16
        v = self.dma_cnt[key]
        self.prog[eng].append(("dma", fn, key))
        self._commit((key, v), reads, writes)

    def barrier(self):
        evs = [(e, self.cnt[e]) for e in CE if self.cnt[e] > 0]
        evs += [(k, v) for k, v in self.dma_cnt.items() if v > 0]
        for eng in ALLENG:
            for k, v in evs:
                if self.seen[eng].get(k, 0) < v:
                    self.prog[eng].append(("wait", k, v))
                    self.seen[eng][k] = v

    def emit(self):
        nc, sems, prog = self.nc, self.sems, self.prog
        with nc.Block() as block:
            def run(e, lst):
                for it in lst:
                    if it[0] == "wait":
                        e.wait_ge(sems[it[1]], it[2])
                    elif it[0] == "op":
                        it[1](e).then_inc(sems[it[2]], 1)
                    else:
                        it[1](e).then_inc(sems[it[2]], 16)

            @block.tensor
            def _(e):
                run(e, prog["pe"])

            @block.scalar
            def _(e):
                run(e, prog["act"])

            @block.vector
            def _(e):
                run(e, prog["dve"])

            @block.gpsimd
            def _(e):
                run(e, prog["pool"])

            @block.sync
            def _(e):
                run(e, prog["sp"])


class Rot:
    def __init__(self, items):
        self.items = list(items)
        self.i = 0

    def next(self):
        it = self.items[self.i % len(self.items)]
        self.i += 1
        return it


C_Q, C_KC, C_VC, C_KS, C_VS, C_KW, C_VW, C_G = 0, 512, 640, 768, 896, 1024, 1152, 1280
C_CB, C_CC, C_CH, C_G1, C_G2 = 1304, 1816, 2328, 2840, 3864
NA = 1304


def build(NSEQ, stop_after=None, dbg=None):
    nc = bass.Bass("TRN2", target_bir_lowering=False)
    es = ExitStack()

    def din(name, shape):
        return nc.dram_tensor(name, list(shape), F32, kind="ExternalInput").ap()

    xT_d = din("xT", [NSEQ, D, S])
    x_d = din("x", [NSEQ, S, D])
    pT_d = din("pT", [NSEQ, 256, S])
    w_in_d = din("w_in", [D, 4888])
    peT_d = din("peT", [2, 64, 32])
    w1_d = din("cmp_w1", [2, 2048, 128])
    b1_d = din("cmp_b1", [128, 2])
    w2_d = din("cmp_w2", [2, 128, 64])
    b2k_d = din("cmp_b2k", [64, 1])
    b2v_d = din("cmp_b2v", [1, 64])
    convw_d = din("conv_wT", [4, 128, 3])
    wnsa_d = din("w_nsa_out", [512, D])
    wcv_d = din("w_conv_out", [512, D])
    wo_d = din("w_o", [D, D])
    ln1g_d = din("ln1_g", [1, D])
    ln1b_d = din("ln1_b", [1, D])
    ln2g_d = din("ln2_g", [1, D])
    ln2b_d = din("ln2_b", [1, D])
    wr_d = din("w_router", [D, 36])
    br_d = din("b_router", [1, 36])
    wg_d = din("expert_w_gate", [32, D, 256])
    wu_d = din("expert_w_up", [32, D, 256])
    wd_d = din("expert_w_down", [32, 256, D])
    pproj_d = din("ple_proj", [256, D])
    pgw_d = din("ple_gate_w", [D, D])
    pgb_d = din("ple_gate_b", [1, D])
    c_ident = din("c_ident", [128, 128])
    c_tri = din("c_tri", [128, 512])
    c_anti = din("c_anti", [128, 512])
    c_cmpmask = din("c_cmpmask", [128, S])
    c_selconst = din("c_selconst", [128, NT * 32])
    c_overlap = din("c_overlap", [128, 32])
    c_eexp = din("c_eexp", [32, S])
    y_d = nc.dram_tensor("y", [NSEQ, S, D], F32, kind="ExternalOutput").ap()
    wg_bf = nc.dram_tensor("wg_bf", [32, D, 256], BF16, kind="Internal").ap()
    wu_bf = nc.dram_tensor("wu_bf", [32, D, 256], BF16, kind="Internal").ap()
    wd_bf = nc.dram_tensor("wd_bf", [32, 256, D], BF16, kind="Internal").ap()
    dbg_d = {}
    if dbg:
        for name, shape in dbg.items():
            dbg_d[name] = nc.dram_tensor("dbg_" + name, list(shape), F32, kind="ExternalOutput").ap()

    sc = Sched(nc, es)

    def sb(name, shape, dt):
        return es.enter_context(nc.sbuf_tensor(name, list(shape), dt))

    NPAGE = 38
    arena = sb("arena", [128, NPAGE * PAGE], BF16)

    def pages(p0, n):
        return arena[:, p0 * PAGE:(p0 + n) * PAGE]

    ident_bf = sb("ident_bf", [128, 128], BF16)
    ident_f = sb("ident_f", [128, 128], F32)
    tri_bf = sb("tri_bf", [128, 512], BF16)
    anti_bf = sb("anti_bf", [128, 512], BF16)
    cmpmask_bf = sb("cmpmask_bf", [128, S], BF16)
    selconst = sb("selconst", [128, NT, 32], F32)
    w2_bf = sb("w2_bf", [128, 2, 64], BF16)
    b1c = sb("b1c", [128, 2], F32)
    const1 = sb("const1", [128, 2], F32)
    b2k = sb("b2k", [64, 1], F32)
    b2v_bf = sb("b2v_bf", [1, 64], BF16)
    ones_bf = sb("ones_bf", [1, 128], BF16)
    peT_bf = sb("peT_bf", [64, 2, 32], BF16)
    convw = sb("convw", [128, 4, 3], F32)
    wr_f = sb("wr_f", [128, 8, 36], F32)
    wr_hi = sb("wr_hi", [128, 8, 36], BF16)
    wr_lo = sb("wr_lo", [128, 8, 36], BF16)
    br_b = sb("br_b", [128, 36], F32)
    pgb_bf = sb("pgb_bf", [1, D], BF16)
    vcaug = sb("vcaug", [128, 2, 97], BF16)
    kcT = sb("kcT", [64, 2, 128], BF16)
    negm = sb("negm", [128, 2, 96], BF16)
    PT = [sb(f"PT{i}", [128, 512], BF16) for i in range(4)]
    tmp = sb("tmp", [128, 12288], BF16)

    def tmpf(off, n):
        return tmp[:, 2 * off:2 * (off + n)].bitcast(F32)

    ps = [es.enter_context(nc.psum_tensor(f"ps{i}", [128, 512], F32)) for i in range(8)]
    psb = [sc.buf(f"ps{i}") for i in range(8)]

    B_const = sc.buf("const")
    B_wbf = [sc.buf() for _ in range(32)]
    cq = Rot(["pool"])

    def pdma(out, in_, key, reads=(), writes=(), maxlast=None):
        if maxlast:
            sc.dma("pool", lambda e: e.dma_start(out=out, in_=in_, max_dma_last_dim=maxlast), key, reads, writes)
        else:
            sc.dma("pool", lambda e: e.dma_start(out=out, in_=in_), key, reads, writes)

    def sdma(out, in_, key, reads=(), writes=()):
        sc.dma("sp", lambda e: e.dma_start(out=out, in_=in_), key, reads, writes)

    def mm(specs):
        def fn(e):
            last = None
            for (o, l, r, st, sp) in specs:
                last = e.matmul(o, lhsT=l, rhs=r, start=st, stop=sp)
            return last
        return fn

    def PE(specs, reads, writes):
        sc.op("pe", mm(specs), reads, writes)

    def ACT(out, in_, func, reads, writes, **kw):
        sc.op("act", lambda e: e.activation(out=out, in_=in_, func=func, **kw), reads, writes)

    def DVE(fn, reads, writes):
        sc.op("dve", fn, reads, writes)

    def POOL(fn, reads, writes):
        sc.op("pool", fn, reads, writes)

    def dump(name, ap_sb, rd, idx=None):
        if name in dbg_d:
            dst = dbg_d[name] if idx is None else dbg_d[name][idx]
            sdma(dst, ap_sb, "dbg", reads=rd)

    K = "cst"
    pdma(ident_bf[:], c_ident[:, :], K, writes=[B_const])
    sdma(ident_f[:], c_ident[:, :], "cst2", writes=[B_const])
    pdma(tri_bf[:], c_tri[:, :], K, writes=[B_const])
    pdma(anti_bf[:], c_anti[:, :], K, writes=[B_const])
    pdma(cmpmask_bf[:], c_cmpmask[:, :], K, writes=[B_const])
    sdma(selconst[:].rearrange("p a b -> p (a b)"), c_selconst[:, :], "cst2", writes=[B_const])
    pdma(w2_bf[:], w2_d.rearrange("k h d -> h k d"), K, writes=[B_const])
    sdma(b1c[:], b1_d[:, :], "cst2", writes=[B_const])
    sdma(b2k[:], b2k_d[:, :], "cst2", writes=[B_const])
    pdma(b2v_bf[:], b2v_d[:, :], K, writes=[B_const])
    pdma(peT_bf[:], peT_d.rearrange("k d l -> d k l"), K, writes=[B_const])
    sdma(convw[:], convw_d.rearrange("c p k -> p c k"), "cst2", writes=[B_const])
    sdma(wr_f[:], wr_d.rearrange("(kc p) n -> p kc n", p=128), "cst2", writes=[B_const])
    sdma(br_b[:], br_d.partition_broadcast(128).rearrange("p o n -> p (o n)"), "cst2", writes=[B_const])
    pdma(pgb_bf[:], pgb_d[:, :], K, writes=[B_const])
    for g in range(2):
        pdma(vcaug[:, g, 65:97], c_overlap[:, :], K, writes=[B_const])
    POOL(lambda e: e.memset(ones_bf[:], 1.0), [], [B_const])
    POOL(lambda e: e.memset(vcaug[:, :, 64:65], 1.0), [], [B_const])
    POOL(lambda e: e.memset(negm[:], 0.0), [], [B_const])
    sc.barrier()
    DVE(lambda e: e.tensor_copy(out=wr_hi[:], in_=wr_f[:]), [B_const], [B_const])
    DVE(lambda e: e.tensor_tensor(out=wr_lo[:], in0=wr_f[:], in1=wr_hi[:], op=ALU.subtract), [B_const], [B_const])
    sc.barrier()

    for seq in range(NSEQ):
        xT = pages(0, 8).rearrange("p (k t) -> p k t", k=8)
        qT = pages(8, 8).rearrange("p (h t) -> p h t", h=8)
        kcmpT = pages(16, 2).rearrange("p (g t) -> p g t", g=2)
        vcmpT = pages(18, 2).rearrange("p (g t) -> p g t", g=2)
        kslcT = pages(20, 2).rearrange("p (g t) -> p g t", g=2)
        kwinT = pages(22, 2).rearrange("p (g t) -> p g t", g=2)
        misc = pages(24, 4)
        vslc = misc[:, 0:2080].rearrange("p (t g c) -> p t g c", t=NT, g=2)
        vwin = misc[:, 2080:4160].rearrange("p (t g c) -> p t g c", t=NT, g=2)
        gates = misc[:, 4160:4160 + 768].bitcast(F32).rearrange("p (t c) -> p t c", t=NT)
        impacc = misc[:, 4928:4928 + 2048].bitcast(F32).rearrange("p (t g m) -> p t g m", t=NT, g=2)
        o_tok = pages(28, 4).rearrange("p (t c) -> p t c", t=NT)
        wA = pages(28, 6)[:, 0:8 * NA].rearrange("p (k c) -> p k c", k=8)
        W1 = pages(34, 4).rearrange("p (k l h) -> p k l h", k=2, l=32)

        B_xT = [sc.buf() for _ in range(8)]
        B_wA = sc.buf()
        B_W1 = sc.buf()
        B_q = [[sc.buf() for _ in range(4)] for _ in range(8)]
        B_qm = [[sc.buf() for _ in range(4)] for _ in range(8)]
        B_kc = [sc.buf() for _ in range(2)]
        B_vc = [sc.buf() for _ in range(2)]
        B_ks = [[sc.buf() for _ in range(4)] for _ in range(2)]
        B_kw = [[sc.buf() for _ in range(4)] for _ in range(2)]
        B_ee = sc.buf()
        B_v = [sc.buf() for _ in range(NT)]
        B_g = [sc.buf() for _ in range(NT)]
        B_imp = [sc.buf() for _ in range(4)]
        B_o = [sc.buf() for _ in range(4)]
        B_PT = [sc.buf() for _ in range(4)]

        for kc in range(8):
            pdma(xT[:, kc, :], xT_d[seq, kc * 128:(kc + 1) * 128, :], f"xT{kc}", writes=[B_xT[kc]])
        pdma(wA, w_in_d.rearrange("(k p) c -> p k c", p=128)[:, :, 0:NA], "wA", writes=[B_wA])
        for k in range(2):
            pdma(W1[0:64, k], w1_d[k].rearrange("(l d) h -> d l h", d=64), "W1", writes=[B_W1])
        pdma(kslcT[64:96, 0, :], c_eexp[:, :], "ee", writes=[B_ee])
        pdma(kslcT[64:96, 1, :], c_eexp[:, :], "ee", writes=[B_ee])
        if seq == 0:
            for ex in range(32):
                pdma(wg_bf[ex].rearrange("(a b) c -> a (b c)", a=128), wg_d[ex].rearrange("(a b) c -> a (b c)", a=128), f"cv{ex}", writes=[B_wbf[ex]])
                pdma(wu_bf[ex].rearrange("(a b) c -> a (b c)", a=128), wu_d[ex].rearrange("(a b) c -> a (b c)", a=128), f"cv{ex}", writes=[B_wbf[ex]])
                pdma(wd_bf[ex].rearrange("(a b) c -> a (b c)", a=128), wd_d[ex].rearrange("(a b) c -> a (b c)", a=128), f"cv{ex}", writes=[B_wbf[ex]])
        DVE(lambda e: e.memset(vslc[:, :, :, 64:65], 1.0), [], B_v)
        DVE(lambda e: e.memset(vwin[:, :, :, 64:65], 1.0), [], B_v)

        rot = Rot(range(8))
        evq = Rot(["act", "dve"])

        def evac(out, in_, reads, writes):
            if evq.next() == "act":
                ACT(out, in_, AF.Copy, reads, writes)
            else:
                DVE(lambda e: e.tensor_copy(out=out, in_=in_), reads, writes)

        fm = []
        for h in range(8):
            fm.append((C_Q + 64 * h, (lambda c, h=h: qT[0:64, h, c * 512:(c + 1) * 512]), (lambda c, h=h: B_q[h][c])))
        for g in range(2):
            fm.append((C_KC + 64 * g, (lambda c, g=g: kcmpT[0:64, g, c * 512:(c + 1) * 512]), (lambda c, g=g: B_kc[g])))
            fm.append((C_VC + 64 * g, (lambda c, g=g: vcmpT[0:64, g, c * 512:(c + 1) * 512]), (lambda c, g=g: B_vc[g])))
            fm.append((C_KS + 64 * g, (lambda c, g=g: kslcT[0:64, g, c * 512:(c + 1) * 512]), (lambda c, g=g: B_ks[g][c])))
            fm.append((C_KW + 64 * g, (lambda c, g=g: kwinT[0:64, g, c * 512:(c + 1) * 512]), (lambda c, g=g: B_kw[g][c])))
        for (c0, dst, dbuf) in fm:
            for c in range(4):
                b = rot.next()
                PE([(ps[b][0:64, :], wA[:, kc, c0:c0 + 64], xT[:, kc, c * 512:(c + 1) * 512], kc == 0, kc == 7)
                    for kc in range(8)], B_xT + [B_wA], [psb[b]])
                evac(dst(c), ps[b][0:64, :], [psb[b]], [dbuf(c)])
        for t in range(NT):
            b = rot.next()
            PE([(ps[b][:, 0:408], xT[:, kc, t * 128:(t + 1) * 128], wA[:, kc, C_VS:C_VS + 408], kc == 0, kc == 7)
                for kc in range(8)], B_xT + [B_wA], [psb[b]])
            ACT(vslc[:, t, :, 0:64], ps[b][:, 0:128].rearrange("p (g c) -> p g c", g=2), AF.Copy, [psb[b]], [B_v[t]])
            DVE(lambda e, b=b, t=t: e.tensor_copy(out=vwin[:, t, :, 0:64],
                                                  in_=ps[b][:, 256:384].rearrange("p (g c) -> p g c", g=2)),
                [psb[b]], [B_v[t]])
            ACT(gates[:, t, :], ps[b][:, 384:408], AF.Sigmoid, [psb[b]], [B_g[t]])
        if dbg and "qT" in dbg_d:
            for h in range(8):
                tq = tmpf(0, 2048)
                Bt = sc.buf()
                DVE(lambda e, h=h: e.tensor_copy(out=tq[0:64, :], in_=qT[0:64, h, :]), B_q[h], [Bt])
                sdma(dbg_d["qT"][h], tq[0:64, :], "dbg", reads=[Bt])
                sc.barrier()
        sc.barrier()
        if stop_after == 1:
            continue

        if seq == 0:
            for k in range(2):
                b = rot.next()
                PE([(ps[b][:, 0:1], W1[0:64, k, l, :], peT_bf[:, k, l:l + 1], l == 0, l == 31) for l in range(32)],
                   [B_W1, B_const], [psb[b]])
                DVE(lambda e, b=b, k=k: e.tensor_tensor(out=const1[:, k:k + 1], in0=ps[b][:, 0:1], in1=b1c[:, k:k + 1], op=ALU.add),
                    [psb[b], B_const], [B_const])
        B_t = [sc.buf() for _ in range(6)]
        cx = tmpf(0, 128)
        ct2 = tmpf(128, 128)
        csg = tmpf(256, 128)
        hT = tmp[:, 1024:1024 + 128]
        for k in range(2):
            src, Bsrc = (kcmpT, B_kc) if k == 0 else (vcmpT, B_vc)
            for g in range(2):
                b = rot.next()
                PE([(ps[b][:, 0:127], W1[0:64, k, l, :], src[0:64, g, l:l + 16 * 126 + 1:16], l == 0, l == 31)
                    for l in range(32)], [B_W1, Bsrc[g]], [psb[b]])
                DVE(lambda e, b=b, k=k: e.tensor_scalar(out=cx[:, 0:127], in0=ps[b][:, 0:127], scalar1=const1[:, k:k + 1],
                                                        scalar2=None, op0=ALU.add), [psb[b], B_const], [B_t[0]])
                DVE(lambda e: e.tensor_tensor(out=ct2[:, 0:127], in0=cx[:, 0:127], in1=cx[:, 0:127], op=ALU.mult),
                    [B_t[0]], [B_t[1]])
                DVE(lambda e: e.tensor_scalar(out=ct2[:, 0:127], in0=ct2[:, 0:127], scalar1=0.044715, scalar2=1.0,
                                              op0=ALU.mult, op1=ALU.add), [B_t[1]], [B_t[1]])
                DVE(lambda e: e.tensor_tensor(out=ct2[:, 0:127], in0=ct2[:, 0:127], in1=cx[:, 0:127], op=ALU.mult),
                    [B_t[0], B_t[1]], [B_t[1]])
                ACT(csg[:, 0:127], ct2[:, 0:127], AF.Sigmoid, [B_t[1]], [B_t[2]], scale=1.5957691216057308)
                DVE(lambda e: e.tensor_tensor(out=hT[:, 0:127], in0=cx[:, 0:127], in1=csg[:, 0:127], op=ALU.mult),
                    [B_t[0], B_t[2]], [B_t[3]])
                b2 = rot.next()
                if k == 0:
                    PE([(ps[b2][0:64, 0:127], w2_bf[:, 0, :], hT[:, 0:127], True, True)], [B_t[3], B_const], [psb[b2]])
                    DVE(lambda e, b2=b2, g=g: e.tensor_scalar(out=kcT[:, g, 0:127], in0=ps[b2][0:64, 0:127], scalar1=b2k[:, 0:1],
                                                              scalar2=None, op0=ALU.add), [psb[b2], B_const], [B_kc[g]])
                else:
                    PE([(ps[b2][0:127, 0:64], hT[:, 0:127], w2_bf[:, 1, :], True, False),
                        (ps[b2][0:127, 0:64], ones_bf[0:1, 0:127], b2v_bf[0:1, :], False, True)],
                       [B_t[3], B_const], [psb[b2]])
                    DVE(lambda e, b2=b2, g=g: e.tensor_copy(out=vcaug[0:127, g, 0:64], in_=ps[b2][0:127, 0:64]),
                        [psb[b2]], [B_vc[g]])
        sc.barrier()

        sbank = Rot([0, 1, 2, 3])
        abank = Rot([4, 5])
        ptrot = Rot(range(4))
        rs = tmpf(0, 4)
        sgate = tmpf(8, 4)
        otmp = tmpf(16, 256)
        itmp = tmpf(272, 128)
        B_rs, B_sg, B_ot, B_it = sc.buf(), sc.buf(), sc.buf(), sc.buf()

        def evac_attn(ab, hh, c, br, ncol, first):
            acc = ps[ab][:, :].rearrange("p (j c) -> p j c", j=4)
            g = hh // 4
            DVE(lambda e: e.tensor_scalar(out=rs[:, 0:4], in0=acc[:, :, 64:65].rearrange("p j o -> p (j o)"), scalar1=1e-30,
                                          scalar2=None, op0=ALU.add), [psb[ab]], [B_rs])
            DVE(lambda e: e.reciprocal(out=rs[:, 0:4], in_=rs[:, 0:4]), [B_rs], [B_rs])
            DVE(lambda e: e.tensor_tensor(out=sgate[:, 0:4], in0=rs[:, 0:4], in1=gates[:, 4 * c:4 * c + 4, hh * 3 + br],
                                          op=ALU.mult), [B_rs] + B_g[4 * c:4 * c + 4], [B_sg])
            dst = o_tok[:, 4 * c:4 * c + 4, hh * 64:(hh + 1) * 64]
            sgb = sgate[:, 0:4].unsqueeze(2).to_broadcast([128, 4, 64])
            if first:
                DVE(lambda e: e.tensor_tensor(out=dst, in0=acc[:, :, 0:64], in1=sgb, op=ALU.mult),
                    [psb[ab], B_sg], [B_o[c]])
            else:
                ot = otmp[:, 0:256].rearrange("p (j c) -> p j c", j=4)
                DVE(lambda e: e.tensor_tensor(out=ot, in0=acc[:, :, 0:64], in1=sgb, op=ALU.mult),
                    [psb[ab], B_sg], [B_ot])
                DVE(lambda e: e.tensor_tensor(out=dst, in0=dst, in1=ot, op=ALU.add), [B_ot, B_o[c]], [B_o[c]])
            if br == 0:
                rsb = rs[:, 0:4].unsqueeze(2).to_broadcast([128, 4, 32])
                idst = impacc[:, 4 * c:4 * c + 4, g, :]
                if hh % 4 == 0:
                    DVE(lambda e: e.tensor_tensor(out=idst, in0=acc[:, :, 65:97], in1=rsb, op=ALU.mult),
                        [psb[ab], B_rs], [B_imp[c]])
                else:
                    it = itmp[:, 0:128].rearrange("p (j c) -> p j c", j=4)
                    DVE(lambda e: e.tensor_tensor(out=it, in0=acc[:, :, 65:97], in1=rsb, op=ALU.mult),
                        [psb[ab], B_rs], [B_it])
                    DVE(lambda e: e.tensor_tensor(out=idst, in0=idst, in1=it, op=ALU.add), [B_it, B_imp[c]], [B_imp[c]])

        for hh in range(8):
            g = hh // 4
            for c in range(4):
                b = sbank.next()
                PE([(ps[b][0:127, :], kcT[:, g, 0:127], qT[0:64, hh, c * 512:(c + 1) * 512], True, False),
                    (ps[b][0:127, :], ident_bf[0:127, 0:127], cmpmask_bf[0:127, c * 512:(c + 1) * 512], False, True)],
                   [B_kc[g], B_q[hh][c], B_const], [psb[b]])
                pi = ptrot.next()
                ACT(PT[pi][0:127, :], ps[b][0:127, :], AF.Exp, [psb[b]], [B_PT[pi]], scale=0.125)
                ab = abank.next()
                PE([(ps[ab][:, j * 128:j * 128 + 97], PT[pi][0:127, j * 128:(j + 1) * 128], vcaug[0:127, g, :], True, True)
                    for j in range(4)], [B_PT[pi], B_vc[g], B_const], [psb[ab]])
                evac_attn(ab, hh, c, 0, 128, True)
        score = tmpf(512, 32)
        work = tmpf(544, 32)
        m8a = tmpf(576, 8)
        m8b = tmpf(584, 8)
        msk = tmpf(592, 32)
        B_s = [sc.buf() for _ in range(5)]
        B_negm = sc.buf()
        for c in range(4):
            for g in range(2):
                b = sbank.next()
                for j in range(4):
                    t = 4 * c + j
                    DVE(lambda e, t=t, g=g: e.tensor_tensor(out=score[:, :], in0=impacc[:, t, g, :], in1=selconst[:, t, :], op=ALU.add),
                        [B_imp[c], B_const], [B_s[0]])
                    DVE(lambda e: e.max(out=m8a[:, :], in_=score[:, :]), [B_s[0]], [B_s[1]])
                    DVE(lambda e: e.match_replace(out=work[:, :], in_to_replace=m8a[:, :], in_values=score[:, :], imm_value=-1e9),
                        [B_s[0], B_s[1]], [B_s[2]])
                    DVE(lambda e: e.max(out=m8b[:, :], in_=work[:, :]), [B_s[2]], [B_s[3]])
                    DVE(lambda e: e.tensor_scalar(out=msk[:, :], in0=score[:, :], scalar1=m8b[:, 7:8], scalar2=None, op0=ALU.is_ge),
                        [B_s[0], B_s[3]], [B_s[4]])
                    DVE(lambda e, g=g: e.tensor_scalar(out=negm[:, g, 64:96], in0=msk[:, :], scalar1=-1.0, scalar2=-NEG,
                                                       op0=ALU.add, op1=ALU.mult), [B_s[4]], [B_negm])
                    PE([(ps[b][0:96, j * 128:(j + 1) * 128], negm[:, g, :], ident_bf[:, :], True, True)],
                       [B_negm, B_const], [psb[b]])
                    if "msk" in dbg_d:
                        sdma(dbg_d["msk"][g, t], msk[:, :], "dbg", reads=[B_s[4]])
                        sc.barrier()
                DVE(lambda e, b=b, g=g, c=c: e.tensor_copy(
                    out=qT[64:96, 4 * g:4 * g + 4, c * 512:(c + 1) * 512],
                    in_=ps[b][64:96, :].unsqueeze(1).to_broadcast([32, 4, 512])),
                    [psb[b]], [B_qm[4 * g + h][c] for h in range(4)])
        sc.barrier()

        def attn_branch(br, kT, Bk, vtok, krows):
            pending = []

            def flush(n):
                while len(pending) > n:
                    pending.pop(0)()
            for hh in range(8):
                g = hh // 4
                for c in range(4):
                    ab = abank.next()
                    kt0 = 0 if br == 1 else max(0, 4 * c - 4)
                    kts = list(range(kt0, 4 * c + 4))
                    for kt in kts:
                        r = kt - 4 * c
                        if r >= 0:
                            j0, j1 = r, 4
                        elif br == 1:
                            j0, j1 = 0, 4
                        else:
                            j0, j1 = 0, r + 5
                        q0, q1 = c * 512 + j0 * 128, c * 512 + j1 * 128
                        N = q1 - q0
                        b = sbank.next()
                        specs = []
                        rd = [Bk[g][kt // 4], B_q[hh][c], B_const]
                        if br == 1:
                            rd += [B_qm[hh][c], B_ee]
                        if r >= 0:
                            specs.append((ps[b][:, 0:N], ident_bf[:, :], tri_bf[:, 0:N], True, False))
                        elif br == 2:
                            specs.append((ps[b][:, 0:N], ident_bf[:, :], anti_bf[:, 512 - N:512], True, False))
                        specs.append((ps[b][:, 0:N], kT[0:krows, g, kt * 128:(kt + 1) * 128], qT[0:krows, hh, q0:q1],
                                      len(specs) == 0, True))
                        PE(specs, rd, [psb[b]])
                        pi = ptrot.next()
                        ACT(PT[pi][:, 0:N], ps[b][:, 0:N], AF.Exp, [psb[b]], [B_PT[pi]], scale=0.125)

                        def att_s2(hh=hh, g=g, c=c, ab=ab, kt=kt, kts=kts, j0=j0, j1=j1, pi=pi):
                            pv = []
                            for j in range(j0, j1):
                                last_kt = 4 * c + j
                                pv.append((ps[ab][:, j * 128:j * 128 + 65], PT[pi][:, (j - j0) * 128:(j - j0 + 1) * 128],
                                           vtok[:, kt, g, :], (kt == kts[0] and j == j0), kt == last_kt))
                            PE(pv, [B_PT[pi], B_v[kt]], [psb[ab]])
                            if kt == kts[-1]:
                                evac_attn(ab, hh, c, br, 128, False)
                        pending.append(att_s2)
                        flush(2)
            flush(0)

        attn_branch(1, kslcT, B_ks, vslc, 96)
        attn_branch(2, kwinT, B_kw, vwin, 64)
        sc.barrier()
        if "o_tok" in dbg_d:
            for t in range(NT):
                tq = tmpf(0, 512)
                DVE(lambda e, t=t: e.tensor_copy(out=tq[:, :], in_=o_tok[:, t, :]), [], [])
                sc.barrier()
                sdma(dbg_d["o_tok"][t * 128:(t + 1) * 128, :], tq[:, :], "dbg")
                sc.barrier()
        if stop_after == 2:
            continue

        oT = pages(8, 4).rearrange("p (i t) -> p i t", i=4)
        B_oT = [sc.buf() for _ in range(4)]
        for c in range(4):
            for i in range(4):
                b = rot.next()
                pb = ps[b][:, :].bitcast(BF16)
                sc.op("pe", (lambda b=b, c=c, i=i, pb=pb: (lambda e: [e.transpose(pb[:, j * 128:(j + 1) * 128], o_tok[:, 4 * c + j, i * 128:(i + 1) * 128], ident_bf[:, :]) for j in range(4)][-1]))(),
                      [B_o[c], B_const], [psb[b]])
                evac(oT[:, i, c * 512:(c + 1) * 512], pb[:, 0:512], [psb[b]], [B_oT[c]])
        sc.barrier()

        buT = pages(12, 4).rearrange("p (i t) -> p i t", i=4)
        mergedT = pages(16, 8).rearrange("p (k t) -> p k t", k=8)
        wcB = pages(24, 6).rearrange("p (k c) -> p k c", k=8)
        wgm = [pages(30, 2).rearrange("p (k c) -> p k c", k=8), pages(32, 2).rearrange("p (k c) -> p k c", k=8)]
        wnsa = pages(34, 2).rearrange("p (k c) -> p k c", k=4)
        wcv = pages(36, 2).rearrange("p (k c) -> p k c", k=4)
        B_wcB, B_wnsa, B_wcv = sc.buf(), sc.buf(), sc.buf()
        B_wgm = [sc.buf(), sc.buf()]
        B_bu = [sc.buf() for _ in range(4)]
        B_mg = [sc.buf() for _ in range(4)]
        w_in_v = w_in_d.rearrange("(k p) c -> p k c", p=128)
        pdma(wcB, w_in_v[:, :, C_CB:C_CB + 1536], "wcB", writes=[B_wcB])
        pdma(wnsa, wnsa_d.rearrange("(k p) c -> p k c", p=128), "wnsa", writes=[B_wnsa])
        pdma(wcv, wcv_d.rearrange("(k p) c -> p k c", p=128), "wcv", writes=[B_wcv])
        c_sb = tmpf(0, 512)
        u = tmpf(512, 514)
        uc = tmpf(1026, 512)
        B_c, B_u, B_uc = sc.buf(), sc.buf(), sc.buf()
        for cb in range(4):
            for c in range(4):
                bb, bc, bh = rot.next(), rot.next(), rot.next()
                for (bk, off) in ((bb, 0), (bc, 512), (bh, 1024)):
                    PE([(ps[bk][:, :], wcB[:, kc, off + cb * 128:off + (cb + 1) * 128], xT[:, kc, c * 512:(c + 1) * 512], kc == 0, kc == 7)
                        for kc in range(8)], B_xT + [B_wcB], [psb[bk]])
                if c == 0:
                    DVE(lambda e: e.memset(u[:, 0:2], 0.0), [B_u], [B_u])
                else:
                    DVE(lambda e: e.tensor_copy(out=u[:, 0:2], in_=u[:, 512:514]), [B_u], [B_u])
                ACT(c_sb[:, :], ps[bc][:, :], AF.Copy, [psb[bc]], [B_c])
                DVE(lambda e, bh=bh: e.tensor_tensor(out=u[:, 2:514], in0=c_sb[:, :], in1=ps[bh][:, :], op=ALU.mult),
                    [B_c, psb[bh]], [B_u])
                DVE(lambda e, cb=cb: e.tensor_scalar(out=uc[:, :], in0=u[:, 0:512], scalar1=convw[:, cb, 0:1], scalar2=None, op0=ALU.mult),
                    [B_u, B_const], [B_uc])
                DVE(lambda e, cb=cb: e.scalar_tensor_tensor(out=uc[:, :], in0=u[:, 1:513], scalar=convw[:, cb, 1:2], in1=uc[:, :],
                                                            op0=ALU.mult, op1=ALU.add), [B_u, B_uc], [B_uc])
                DVE(lambda e, cb=cb: e.scalar_tensor_tensor(out=uc[:, :], in0=u[:, 2:514], scalar=convw[:, cb, 2:3], in1=uc[:, :],
                                                            op0=ALU.mult, op1=ALU.add), [B_u, B_uc], [B_uc])
                DVE(lambda e, bb=bb, cb=cb, c=c: e.tensor_tensor(out=buT[:, cb, c * 512:(c + 1) * 512], in0=uc[:, :], in1=ps[bb][:, :], op=ALU.mult),
                    [B_uc, psb[bb]], [B_bu[c]])
        s1 = tmpf(0, 512)
        s2 = tmpf(512, 512)
        m1 = tmpf(1024, 512)
        m2 = tmpf(1536, 512)
        B_s1, B_s2, B_m1, B_m2 = sc.buf(), sc.buf(), sc.buf(), sc.buf()
        for dc in range(8):
            wq = wgm[dc % 2]
            pdma(wq[:, :, 0:128], w_in_v[:, :, C_G1 + dc * 128:C_G1 + (dc + 1) * 128], f"wgm{dc % 2}", writes=[B_wgm[dc % 2]])
            pdma(wq[:, :, 128:256], w_in_v[:, :, C_G2 + dc * 128:C_G2 + (dc + 1) * 128], f"wgm{dc % 2}", writes=[B_wgm[dc % 2]])
            for c in range(4):
                b1, b2, b3, b4 = rot.next(), rot.next(), rot.next(), rot.next()
                tok = slice(c * 512, (c + 1) * 512)
                PE([(ps[b1][:, :], wq[:, kc, 0:128], xT[:, kc, tok], kc == 0, kc == 7) for kc in range(8)],
                   B_xT + [B_wgm[dc % 2]], [psb[b1]])
                PE([(ps[b2][:, :], wq[:, kc, 128:256], xT[:, kc, tok], kc == 0, kc == 7) for kc in range(8)],
                   B_xT + [B_wgm[dc % 2]], [psb[b2]])
                PE([(ps[b3][:, :], wnsa[:, i, dc * 128:(dc + 1) * 128], oT[:, i, tok], i == 0, i == 3) for i in range(4)],
                   [B_wnsa, B_oT[c]], [psb[b3]])
                PE([(ps[b4][:, :], wcv[:, i, dc * 128:(dc + 1) * 128], buT[:, i, tok], i == 0, i == 3) for i in range(4)],
                   [B_wcv, B_bu[c]], [psb[b4]])
                ACT(s1[:, :], ps[b1][:, :], AF.Sigmoid, [psb[b1]], [B_s1])
                ACT(s2[:, :], ps[b2][:, :], AF.Sigmoid, [psb[b2]], [B_s2])
                DVE(lambda e, b3=b3: e.tensor_tensor(out=m1[:, :], in0=s1[:, :], in1=ps[b3][:, :], op=ALU.mult),
                    [B_s1, psb[b3]], [B_m1])
                DVE(lambda e, b4=b4: e.tensor_tensor(out=m2[:, :], in0=s2[:, :], in1=ps[b4][:, :], op=ALU.mult),
                    [B_s2, psb[b4]], [B_m2])
                DVE(lambda e, dc=dc, tok=tok: e.tensor_tensor(out=mergedT[:, dc, tok], in0=m1[:, :], in1=m2[:, :], op=ALU.add),
                    [B_m1, B_m2], [B_mg[c]])
        sc.barrier()
        if stop_after == 3:
            continue

        pT_bf = pages(13, 2).rearrange("p (k t) -> p k t", k=2)
        B_pT = sc.buf()
        pdma(pT_bf, pT_d[seq].rearrange("(k p) t -> p k t", p=128), "pT", writes=[B_pT])
        lnt = pages(9, 4).bitcast(F32).rearrange("p (a d) -> p a d", a=4)
        B_ln = sc.buf()
        for a, src in enumerate((ln1g_d, ln1b_d, ln2g_d, ln2b_d)):
            sdma(lnt[:, a, :], src.partition_broadcast(128).rearrange("p o d -> p (o d)"), "ln", writes=[B_ln])
        pproj = pages(8, 1).rearrange("p (k c) -> p k c", k=2)
        B_pproj = sc.buf()
        pdma(pproj, pproj_d.rearrange("(k p) c -> p k c", p=128), "pproj", writes=[B_pproj])
        x1T = arena[:, 15 * PAGE:16 * PAGE]
        x1T = [arena[:, 15 * PAGE:16 * PAGE].rearrange("p (k t) -> p k t", k=2),
               pages(24, 3).rearrange("p (k t) -> p k t", k=6)]

        def x1T_ap(kc, t0, t1):
            return x1T[0][:, kc, t0:t1] if kc < 2 else x1T[1][:, kc - 2, t0:t1]
        y_acc = pages(27, 8).bitcast(F32).rearrange("p (t d) -> p t d", t=8)
        comb_all = tmpf(4864, 256).rearrange("p (t e) -> p t e", t=8)
        for half in range(2):
            wo = pages(0, 4).rearrange("p (k c) -> p k c", k=8)
            pgw = pages(4, 4).rearrange("p (k c) -> p k c", k=8)
            B_wo, B_pgw = sc.buf(), sc.buf()
            pdma(wo, wo_d.rearrange("(k p) c -> p k c", p=128), "wo", writes=[B_wo])
            pdma(pgw, pgw_d.rearrange("(k p) c -> p k c", p=128), "pgw", writes=[B_pgw])
            B_x1T = [sc.buf() for _ in range(8)]
            B_y = [sc.buf() for _ in range(8)]
            B_comb = [sc.buf() for _ in range(8)]
            xt = [tmpf(0, 1024), tmpf(1024, 1024)]
            B_xt = [sc.buf(), sc.buf()]
            h1 = tmpf(2048, 1024)
            x_hi = tmp[:, 6144:7168]
            x_lo = tmp[:, 7168:8192]
            x1Tlo = tmp[:, 10240:11264].rearrange("p (k t) -> p k t", k=8)
            B_xhi, B_xlo = sc.buf(), sc.buf()
            sgt = tmpf(4096, 512)
            st6 = tmpf(4608, 16)
            mv = tmpf(4624, 4)
            rt = tmpf(4640, 96)
            B_h1, B_x1Tf, B_sgt, B_st, B_mv = sc.buf(), sc.buf(), sc.buf(), sc.buf(), sc.buf()
            B_rt = [sc.buf() for _ in range(12)]

            def layernorm(src, Bsrc, dst, Bdst, ga, ba):
                for hf in range(2):
                    DVE(lambda e, hf=hf: e.bn_stats(out=st6[:, hf * 6:(hf + 1) * 6], in_=src[:, hf * 512:(hf + 1) * 512]),
                        [Bsrc], [B_st])
                DVE(lambda e: e.bn_aggr(out=mv[:, 0:2], in_=st6[:, 0:12]), [B_st], [B_mv])
                DVE(lambda e: e.tensor_scalar(out=mv[:, 1:2], in0=mv[:, 1:2], scalar1=EPS, scalar2=None, op0=ALU.add),
                    [B_mv], [B_mv])
                ACT(mv[:, 1:2], mv[:, 1:2], AF.Sqrt, [B_mv], [B_mv])
                DVE(lambda e: e.reciprocal(out=mv[:, 1:2], in_=mv[:, 1:2]), [B_mv], [B_mv])
                DVE(lambda e: e.tensor_scalar(out=dst, in0=src, scalar1=mv[:, 0:1], scalar2=mv[:, 1:2], op0=ALU.subtract, op1=ALU.mult),
                    [Bsrc, B_mv], [Bdst])
                DVE(lambda e: e.tensor_tensor(out=dst, in0=dst, in1=lnt[:, ga, :], op=ALU.mult), [Bdst, B_ln], [Bdst])
                DVE(lambda e: e.tensor_tensor(out=dst, in0=dst, in1=lnt[:, ba, :], op=ALU.add), [Bdst, B_ln], [Bdst])

            for tl in range(8):
                t = half * 8 + tl
                tsl = slice(t * 128, (t + 1) * 128)
                xb = tl % 2
                sdma(xt[xb][:, :], x_d[seq, tsl, :], f"xt{xb}", writes=[B_xt[xb]])
                bm = (rot.next(), rot.next())
                for hf in range(2):
                    PE([(ps[bm[hf]][:, :], mergedT[:, dc, tsl], wo[:, dc, hf * 512:(hf + 1) * 512], dc == 0, dc == 7) for dc in range(8)],
                       [B_mg[t // 4], B_wo], [psb[bm[hf]]])
                    DVE(lambda e, hf=hf, b=bm[hf], xb=xb: e.scalar_tensor_tensor(
                        out=h1[:, hf * 512:(hf + 1) * 512], in0=xt[xb][:, hf * 512:(hf + 1) * 512], scalar=ALPHA,
                        in1=ps[b][:, :], op0=ALU.mult, op1=ALU.add), [B_xt[xb], psb[bm[hf]]], [B_h1])
                layernorm(h1[:, :], B_h1, h1[:, :], B_h1, 0, 1)
                if "x1" in dbg_d:
                    sdma(dbg_d["x1"][tsl, :], h1[:, :], "dbg", reads=[B_h1])
                DVE(lambda e: e.tensor_copy(out=x_hi[:, :], in_=h1[:, :]), [B_h1], [B_xhi])
                DVE(lambda e: e.tensor_tensor(out=x_lo[:, :], in0=h1[:, :], in1=x_hi[:, :], op=ALU.subtract), [B_h1, B_xhi], [B_xlo])
                bt = (rot.next(), rot.next())
                for which, (srcx, Bsrcx) in enumerate(((x_hi, B_xhi), (x_lo, B_xlo))):
                    b = bt[which]
                    pb = ps[b][:, :].bitcast(BF16)
                    sc.op("pe", (lambda pb=pb, srcx=srcx: (lambda e: [e.transpose(pb[:, j * 128:(j + 1) * 128], srcx[:, j * 128:(j + 1) * 128], ident_bf[:, :]) for j in range(8)][-1]))(),
                          [Bsrcx, B_const], [psb[b]])
                    if which == 0:
                        ACT(x1T[0][:, :, tl * 128:(tl + 1) * 128], pb[:, 0:256].rearrange("p (k t) -> p k t", k=2), AF.Copy,
                            [psb[b]], [B_x1T[tl]])
                        DVE(lambda e, pb=pb, tl=tl: e.tensor_copy(out=x1T[1][:, :, tl * 128:(tl + 1) * 128],
                                                                    in_=pb[:, 256:1024].rearrange("p (k t) -> p k t", k=6)),
                            [psb[b]], [B_x1T[tl]])
                    else:
                        ACT(x1Tlo[:, :, :], pb[:, :].rearrange("p (k t) -> p k t", k=8), AF.Copy, [psb[b]], [B_x1Tf])
                br_ = rot.next()
                specs = []
                for kc in range(8):
                    specs.append((ps[br_][:, 0:36], x1T_ap(kc, tl * 128, (tl + 1) * 128), wr_hi[:, kc, :], kc == 0, False))
                    specs.append((ps[br_][:, 0:36], x1Tlo[:, kc, :], wr_hi[:, kc, :], False, False))
                    specs.append((ps[br_][:, 0:36], x1T_ap(kc, tl * 128, (tl + 1) * 128), wr_lo[:, kc, :], False, kc == 7))
                PE(specs, [B_x1Tf, B_x1T[tl], B_const], [psb[br_]])
                lg = rt[:, 0:36]
                gmax, gsum, oh, ch, m8, wv, w8a, w8b, wg8 = (rt[:, 36:37], rt[:, 37:38], rt[:, 40:44], rt[:, 44:52], rt[:, 52:60],
                                                            rt[:, 60:62], rt[:, 64:72], rt[:, 72:80], rt[:, 80:88])
                gex = rt[:, 88:92]
                DVE(lambda e, b=br_: e.tensor_tensor(out=lg, in0=ps[b][:, 0:36], in1=br_b[:, :], op=ALU.add), [psb[br_], B_const], [B_rt[0]])
                DVE(lambda e: e.tensor_reduce(out=gmax, in_=lg[:, 0:4], axis=AX.X, op=ALU.max), [B_rt[0]], [B_rt[1]])
                DVE(lambda e: e.tensor_scalar(out=oh, in0=lg[:, 0:4], scalar1=gmax, scalar2=None, op0=ALU.is_ge), [B_rt[0], B_rt[1]], [B_rt[2]])
                DVE(lambda e: e.tensor_scalar(out=gex, in0=lg[:, 0:4], scalar1=gmax, scalar2=None, op0=ALU.subtract), [B_rt[0], B_rt[1]], [B_rt[3]])
                ACT(gex, gex, AF.Exp, [B_rt[3]], [B_rt[3]])
                DVE(lambda e: e.tensor_reduce(out=gsum, in_=gex, axis=AX.X, op=ALU.add), [B_rt[3]], [B_rt[4]])
                DVE(lambda e: e.reciprocal(out=gsum, in_=gsum), [B_rt[4]], [B_rt[4]])
                DVE(lambda e: e.tensor_scalar(out=oh, in0=oh, scalar1=gsum, scalar2=None, op0=ALU.mult), [B_rt[2], B_rt[4]], [B_rt[2]])
                ohu = rt[:, 92:96]
                DVE(lambda e: e.tensor_scalar(out=ohu, in0=lg[:, 0:4], scalar1=gmax, scalar2=None, op0=ALU.is_ge), [B_rt[0], B_rt[1]], [B_rt[5]])
                DVE(lambda e: e.tensor_scalar(out=ch, in0=lg[:, 4:12], scalar1=ohu[:, 0:1], scalar2=None, op0=ALU.mult), [B_rt[0], B_rt[5]], [B_rt[6]])
                for g in range(1, 4):
                    DVE(lambda e, g=g: e.scalar_tensor_tensor(out=ch, in0=lg[:, 4 + 8 * g:12 + 8 * g], scalar=ohu[:, g:g + 1], in1=ch,
                                                              op0=ALU.mult, op1=ALU.add), [B_rt[0], B_rt[5], B_rt[6]], [B_rt[6]])
                DVE(lambda e: e.max(out=m8, in_=ch), [B_rt[6]], [B_rt[7]])
                DVE(lambda e: e.tensor_tensor(out=wv[:, 0:1], in0=m8[:, 0:1], in1=m8[:, 1:2], op=ALU.subtract), [B_rt[7]], [B_rt[8]])
                ACT(wv[:, 0:1], wv[:, 0:1], AF.Sigmoid, [B_rt[8]], [B_rt[8]])
                DVE(lambda e: e.tensor_scalar(out=wv[:, 1:2], in0=wv[:, 0:1], scalar1=-1.0, scalar2=1.0, op0=ALU.mult, op1=ALU.add), [B_rt[8]], [B_rt[8]])
                DVE(lambda e: e.tensor_scalar(out=w8a, in0=ch, scalar1=m8[:, 0:1], scalar2=wv[:, 0:1], op0=ALU.is_equal, op1=ALU.mult),
                    [B_rt[6], B_rt[7], B_rt[8]], [B_rt[9]])
                DVE(lambda e: e.tensor_scalar(out=w8b, in0=ch, scalar1=m8[:, 1:2], scalar2=wv[:, 1:2], op0=ALU.is_equal, op1=ALU.mult),
                    [B_rt[6], B_rt[7], B_rt[8]], [B_rt[10]])
                DVE(lambda e: e.tensor_tensor(out=wg8, in0=w8a, in1=w8b, op=ALU.add), [B_rt[9], B_rt[10]], [B_rt[11]])
                DVE(lambda e, tl=tl: e.tensor_tensor(out=comb_all[:, tl, :].rearrange("p (g x) -> p g x", g=4),
                                                     in0=oh.unsqueeze(2).to_broadcast([128, 4, 8]),
                                                     in1=wg8.unsqueeze(1).to_broadcast([128, 4, 8]), op=ALU.mult),
                    [B_rt[2], B_rt[11]], [B_comb[tl]])
                if "comb" in dbg_d:
                    sdma(dbg_d["comb"][tsl, :], comb_all[:, tl, :], "dbg", reads=[B_comb[tl]])
                for hf in range(2):
                    bg, bp = rot.next(), rot.next()
                    cs = slice(hf * 512, (hf + 1) * 512)
                    PE([(ps[bg][:, :], x1T_ap(kc, tl * 128, (tl + 1) * 128), pgw[:, kc, cs], kc == 0, False) for kc in range(8)]
                       + [(ps[bg][:, :], ones_bf[0:1, :], pgb_bf[0:1, cs], False, True)],
                       [B_x1T[tl], B_pgw, B_const], [psb[bg]])
                    PE([(ps[bp][:, :], pT_bf[:, k2, tsl], pproj[:, k2, cs], k2 == 0, k2 == 1) for k2 in range(2)],
                       [B_pT, B_pproj], [psb[bp]])
                    ACT(sgt[:, :], ps[bg][:, :], AF.Sigmoid, [psb[bg]], [B_sgt])
                    DVE(lambda e, bp=bp: e.tensor_tensor(out=sgt[:, :], in0=sgt[:, :], in1=ps[bp][:, :], op=ALU.mult), [B_sgt, psb[bp]], [B_sgt])
                    DVE(lambda e, tl=tl, cs=cs: e.scalar_tensor_tensor(out=y_acc[:, tl, cs], in0=h1[:, cs], scalar=ALPHA, in1=sgt[:, :],
                                                                        op0=ALU.mult, op1=ALU.add), [B_h1, B_sgt], [B_y[tl]])
            sc.barrier()
            if stop_after == 4:
                continue
            wgu = [pages(0, 2).rearrange("p (k c) -> p k c", k=8), pages(3, 2).rearrange("p (k c) -> p k c", k=8)]
            wdn = [pages(2, 1).rearrange("p (k c) -> p k c", k=2), pages(5, 1).rearrange("p (k c) -> p k c", k=2)]
            B_wgu, B_wdn = [sc.buf(), sc.buf()], [sc.buf(), sc.buf()]
            sg_t = [tmp[:, 0:256], tmp[:, 256:512]]
            h_t = [tmp[:, 512:768], tmp[:, 768:1024]]
            hT_t = [tmp[:, 1024:1280].rearrange("p (j t) -> p j t", j=2), tmp[:, 1280:1536].rearrange("p (j t) -> p j t", j=2)]
            B_sgm, B_hm, B_hTm = [sc.buf(), sc.buf()], [sc.buf(), sc.buf()], [sc.buf(), sc.buf()]
            gub = Rot([0, 1])
            htb = Rot([2, 3])
            yb = Rot([(4, 5), (6, 7)])
            it = 0
            pend = []
            for ex in range(32):
                wb = ex % 2
                sdma(wgu[wb][:, :, 0:256], wg_bf[ex].rearrange("(k p) c -> p k c", p=128), f"wg{wb}", reads=[B_wbf[ex]], writes=[B_wgu[wb]])
                sdma(wgu[wb][:, :, 256:512], wu_bf[ex].rearrange("(k p) c -> p k c", p=128), f"wg{wb}", reads=[B_wbf[ex]], writes=[B_wgu[wb]])
                sdma(wdn[wb], wd_bf[ex].rearrange("(k p) c -> p k c", p=128), f"wd{wb}", reads=[B_wbf[ex]], writes=[B_wdn[wb]])
                for tl in range(8):
                    r2 = it % 2
                    it += 1
                    bgu = gub.next()
                    PE([(ps[bgu][:, :], x1T_ap(kc, tl * 128, (tl + 1) * 128), wgu[wb][:, kc, :], kc == 0, kc == 7) for kc in range(8)],
                       [B_x1T[tl], B_wgu[wb]], [psb[bgu]])
                    ACT(sg_t[r2][:, :], ps[bgu][:, 0:256], AF.Silu, [psb[bgu]], [B_sgm[r2]])
                    DVE(lambda e, bgu=bgu, r2=r2, tl=tl, ex=ex: e.scalar_tensor_tensor(
                        out=h_t[r2][:, :], in0=ps[bgu][:, 256:512], scalar=comb_all[:, tl, ex:ex + 1], in1=sg_t[r2][:, :],
                        op0=ALU.mult, op1=ALU.mult), [psb[bgu], B_comb[tl], B_sgm[r2]], [B_hm[r2]])

                    def moe_s2(r2=r2, tl=tl, wb=wb):
                        bh_ = htb.next()
                        pb = ps[bh_][:, :].bitcast(BF16)
                        sc.op("pe", (lambda r2=r2, pb=pb: (lambda e: [e.transpose(pb[:, j * 128:(j + 1) * 128], h_t[r2][:, j * 128:(j + 1) * 128], ident_bf[:, :]) for j in range(2)][-1]))(),
                              [B_hm[r2], B_const], [psb[bh_]])
                        ACT(hT_t[r2], pb[:, 0:256].rearrange("p (j t) -> p j t", j=2), AF.Copy, [psb[bh_]], [B_hTm[r2]])
                        by = yb.next()
                        for hf in range(2):
                            PE([(ps[by[hf]][:, :], hT_t[r2][:, j, :], wdn[wb][:, j, hf * 512:(hf + 1) * 512], j == 0, j == 1) for j in range(2)],
                               [B_hTm[r2], B_wdn[wb]], [psb[by[hf]]])
                            DVE(lambda e, tl=tl, hf=hf, b=by[hf]: e.tensor_tensor(out=y_acc[:, tl, hf * 512:(hf + 1) * 512],
                                                                                   in0=y_acc[:, tl, hf * 512:(hf + 1) * 512],
                                                                                   in1=ps[b][:, :], op=ALU.add),
                                [psb[by[hf]], B_y[tl]], [B_y[tl]])
                    pend.append(moe_s2)
                    while len(pend) > 1:
                        pend.pop(0)()
            while pend:
                pend.pop(0)()
            if stop_after == 5:
                sc.barrier()
                continue
            for tl in range(8):
                t = half * 8 + tl
                layernorm(y_acc[:, tl, :], B_y[tl], y_acc[:, tl, :], B_y[tl], 2, 3)
                sdma(y_d[seq, t * 128:(t + 1) * 128, :], y_acc[:, tl, :], f"yo{tl % 2}", reads=[B_y[tl]])
            sc.barrier()

    sc.barrier()
    sc.emit()
    es.close()
    return nc


def _consts():
    c = {}
    c["c_ident"] = np.eye(128, dtype=np.float32)
    a = np.arange(128)
    tri = np.zeros((128, 512), np.float32)
    tri[:, 0:128] = np.where(a[:, None] <= a[None, :], 0.0, NEG)
    c["c_tri"] = tri
    anti = np.zeros((128, 512), np.float32)
    anti[:, 384:512] = np.where(a[:, None] > a[None, :], 0.0, NEG)
    c["c_anti"] = anti
    n = np.arange(128)
    t = np.arange(S)
    c["c_cmpmask"] = np.where(n[:, None] * 16 + 31 <= t[None, :], 0.0, NEG).astype(np.float32)
    tt = (np.arange(NT)[None, :, None] * 128 + np.arange(128)[:, None, None])
    j = np.arange(32)[None, None, :]
    cur = tt // 64
    valid = j * 64 <= tt
    forced = (j == 0) | (j == cur) | (j == cur - 1)
    c["c_selconst"] = np.where(valid, 1e4 * forced, -1e4).astype(np.float32).reshape(128, NT * 32)
    cs = np.arange(128) * 16
    ce = cs + 31
    ss = np.arange(32) * 64
    se = ss + 63
    ov = ((cs[:, None] <= se[None, :]) & (ce[:, None] >= ss[None, :])).astype(np.float32)
    ov[127] = 0.0
    c["c_overlap"] = ov
    c["c_eexp"] = (np.arange(S)[None, :] // 64 == np.arange(32)[:, None]).astype(np.float32)
    return c


def _weights(inp):
    w = {}
    f = lambda a: np.ascontiguousarray(a, dtype=np.float32)
    w["w_in"] = f(inp["w_in"][0])
    w["peT"] = f(np.transpose(inp["cmp_pe"][0], (0, 2, 1)))
    w["cmp_w1"] = f(inp["cmp_w1"][0])
    w["cmp_b1"] = f(np.transpose(inp["cmp_b1"][0], (1, 0)))
    w["cmp_w2"] = f(inp["cmp_w2"][0])
    w["cmp_b2k"] = f(inp["cmp_b2"][0, 0].reshape(64, 1))
    w["cmp_b2v"] = f(inp["cmp_b2"][0, 1].reshape(1, 64))
    w["conv_wT"] = f(np.transpose(inp["conv_w"][0], (1, 0)).reshape(4, 128, 3))
    w["w_nsa_out"] = f(inp["w_nsa_out"][0])
    w["w_conv_out"] = f(inp["w_conv_out"][0])
    w["w_o"] = f(inp["w_o"][0])
    for k in ("ln1_g", "ln1_b", "ln2_g", "ln2_b", "ple_gate_b"):
        w[k] = f(inp[k][0].reshape(1, D))
    w["w_router"] = f(np.concatenate([inp["router_group_w"][0], inp["router_expert_w"][0]], axis=1))
    w["b_router"] = f(np.concatenate([inp["router_group_b"][0], inp["router_expert_b"][0]], axis=0).reshape(1, 36))
    w["expert_w_gate"] = f(inp["expert_w_gate"][0])
    w["expert_w_up"] = f(inp["expert_w_up"][0])
    w["expert_w_down"] = f(inp["expert_w_down"][0])
    w["ple_proj"] = f(inp["ple_proj"][0])
    w["ple_gate_w"] = f(inp["ple_gate_w"][0])
    return w


def kernel(**inputs):
    inp = {k: np.asarray(v) for k, v in inputs.items()}
    x = inp["x"]
    p = inp["p"][0]
    B = x.shape[0]
    nseq = B // NCORES
    shared = _weights(inp)
    shared.update(_consts())
    in_maps = []
    for c in range(NCORES):
        xs = x[c * nseq:(c + 1) * nseq]
        m = dict(shared)
        m["x"] = np.ascontiguousarray(xs)
        m["xT"] = np.ascontiguousarray(np.transpose(xs, (0, 2, 1)))
        m["pT"] = np.ascontiguousarray(np.transpose(p[c * nseq:(c + 1) * nseq], (0, 2, 1)))
        in_maps.append(m)
    nc = build(nseq)
    res = run_bass_kernel_spmd(nc, in_maps, core_ids=list(range(NCORES)))
    out = np.concatenate([np.asarray(r["y"]) for r in res.results], axis=0)
    return out.astype(np.float32, copy=False)
```

```python
from contextlib import ExitStack
import numpy as np
import concourse.bass as bass
import concourse.mybir as mybir
from concourse.bass_utils import run_bass_kernel_spmd

F32 = mybir.dt.float32
BF16 = mybir.dt.bfloat16
AF = mybir.ActivationFunctionType
ALU = mybir.AluOpType
AX = mybir.AxisListType

NCORES = 8
S = 2048
D = 1024
NT = 16
ALPHA = 2.0 ** 0.25
EPS = 1e-5
NEG = -30000.0
PAGE = 2048

CE = ("pe", "act", "dve", "pool")
ALLENG = ("pe", "act", "dve", "pool", "sp")


class Buf:
    __slots__ = ("name", "w", "r")

    def __init__(self, name):
        self.name = name
        self.w = None
        self.r = {}


class Sched:
    def __init__(self, nc, es):
        self.nc = nc
        self.es = es
        self.prog = {e: [] for e in ALLENG}
        self.sems = {}
        for e in CE:
            self.sems[e] = es.enter_context(nc.semaphore("c_" + e))
        self.cnt = {e: 0 for e in CE}
        self.seen = {e: {} for e in ALLENG}
        self.clock = {e: [None] for e in CE}
        self.dma_cnt = {}

    def buf(self, name="b"):
        return Buf(name)

    def _need(self, eng, reads, writes):
        need = {}

        def add(k, v):
            if need.get(k, 0) < v:
                need[k] = v
        for b in reads:
            if b.w is not None:
                add(*b.w)
        for b in writes:
            if b.w is not None:
                add(*b.w)
            for k, v in b.r.items():
                add(k, v)
        seen = self.seen[eng]
        for k, v in need.items():
            if k == eng and eng == "pe":
                continue
            if seen.get(k, 0) >= v:
                continue
            self.prog[eng].append(("wait", k, v))
            seen[k] = v
            if k in CE:
                snap = self.clock[k][v]
                if snap:
                    for k2, v2 in snap.items():
                        if seen.get(k2, 0) < v2:
                            seen[k2] = v2

    def _commit(self, ev, reads, writes):
        k, v = ev
        for b in reads:
            if b.r.get(k, 0) < v:
                b.r[k] = v
        for b in writes:
            b.w = ev
            b.r = {}

    def op(self, eng, fn, reads=(), writes=()):
        self._need(eng, reads, writes)
        self.cnt[eng] += 1
        v = self.cnt[eng]
        self.prog[eng].append(("op", fn, eng))
        self.clock[eng].append({k: vv for k, vv in self.seen[eng].items() if k in CE})
        self._commit((eng, v), reads, writes)

    def dma(self, eng, fn, key, reads=(), writes=()):
        self._need(eng, reads, writes)
        if key not in self.sems:
            self.sems[key] = self.es.enter_context(self.nc.semaphore("d_" + key))
            self.dma_cnt[key] = 0
        self.dma_cnt[key] += 16
        v = self.dma_cnt[key]
        self.prog[eng].append(("dma", fn, key))
        self._commit((key, v), reads, writes)

    def barrier(self):
        evs = [(e, self.cnt[e]) for e in CE if self.cnt[e] > 0]
        evs += [(k, v) for k, v in self.dma_cnt.items() if v > 0]
        for eng in ALLENG:
            for k, v in evs:
                if self.seen[eng].get(k, 0) < v:
                    self.prog[eng].append(("wait", k, v))
                    self.seen[eng][k] = v

    def emit(self):
        nc, sems, prog = self.nc, self.sems, self.prog
        with nc.Block() as block:
            def run(e, lst):
                for it in lst:
                    if it[0] == "wait":
                        e.wait_ge(sems[it[1]], it[2])
                    elif it[0] == "op":
                        it[1](e).then_inc(sems[it[2]], 1)
                    else:
                        it[1](e).then_inc(sems[it[2]], 16)

            @block.tensor
            def _(e):
                run(e, prog["pe"])

            @block.scalar
            def _(e):
                run(e, prog["act"])

            @block.vector
            def _(e):
                run(e, prog["dve"])

            @block.gpsimd
            def _(e):
                run(e, prog["pool"])

            @block.sync
            def _(e):
                run(e, prog["sp"])


class Rot:
    def __init__(self, items):
        self.items = list(items)
        self.i = 0

    def next(self):
        it = self.items[self.i % len(self.items)]
        self.i += 1
        return it


C_Q, C_KC, C_VC, C_KS, C_VS, C_KW, C_VW, C_G = 0, 512, 640, 768, 896, 1024, 1152, 1280
C_CB, C_CC, C_CH, C_G1, C_G2 = 1304, 1816, 2328, 2840, 3864
NA = 1304


def build(NSEQ, stop_after=None, dbg=None):
    nc = bass.Bass("TRN2", target_bir_lowering=False)
    es = ExitStack()

    def din(name, shape):
        return nc.dram_tensor(name, list(shape), F32, kind="ExternalInput").ap()

    xT_d = din("xT", [NSEQ, D, S])
    x_d = din("x", [NSEQ, S, D])
    pT_d = din("pT", [NSEQ, 256, S])
    w_in_d = din("w_in", [D, 4888])
    peT_d = din("peT", [2, 64, 32])
    w1_d = din("cmp_w1", [2, 2048, 128])
    b1_d = din("cmp_b1", [128, 2])
    w2_d = din("cmp_w2", [2, 128, 64])
    b2k_d = din("cmp_b2k", [64, 1])
    b2v_d = din("cmp_b2v", [1, 64])
    convw_d = din("conv_wT", [4, 128, 3])
    wnsa_d = din("w_nsa_out", [512, D])
    wcv_d = din("w_conv_out", [512, D])
    wo_d = din("w_o", [D, D])
    ln1g_d = din("ln1_g", [1, D])
    ln1b_d = din("ln1_b", [1, D])
    ln2g_d = din("ln2_g", [1, D])
    ln2b_d = din("ln2_b", [1, D])
    wr_d = din("w_router", [D, 36])
    br_d = din("b_router", [1, 36])
    wg_d = din("expert_w_gate", [32, D, 256])
    wu_d = din("expert_w_up", [32, D, 256])
    wd_d = din("expert_w_down", [32, 256, D])
    pproj_d = din("ple_proj", [256, D])
    pgw_d = din("ple_gate_w", [D, D])
    pgb_d = din("ple_gate_b", [1, D])
    c_ident = din("c_ident", [128, 128])
    c_tri = din("c_tri", [128, 512])
    c_anti = din("c_anti", [128, 512])
    c_cmpmask = din("c_cmpmask", [128, S])
    c_selconst = din("c_selconst", [128, NT * 32])
    c_overlap = din("c_overlap", [128, 32])
    c_eexp = din("c_eexp", [32, S])
    y_d = nc.dram_tensor("y", [NSEQ, S, D], F32, kind="ExternalOutput").ap()
    wgu_tab = nc.dram_tensor("wgu_tab", [32 * 128, 8 * 512], BF16, kind="Internal").ap()
    wd_tab = nc.dram_tensor("wd_tab", [32 * 128, 2 * 1024], BF16, kind="Internal").ap()
    xs_d = nc.dram_tensor("xs_d", [8192, D], BF16, kind="Internal").ap()
    ys_d = nc.dram_tensor("ys_d", [8192, D], F32, kind="Internal").ap()
    yinit_d = nc.dram_tensor("yinit_d", [S, D], F32, kind="Internal").ap()
    c_ones = din("c_ones", [128, 128])
    c_lstrict = din("c_lstrict", [128, 128])
    c_thr128 = din("c_thr128", [128, 32])
    c_sval = din("c_sval", [128, 64])
    c_pidx = din("c_pidx", [128, 1])
    dbg_d = {}
    if dbg:
        for name, shape in dbg.items():
            dbg_d[name] = nc.dram_tensor("dbg_" + name, list(shape), F32, kind="ExternalOutput").ap()

    sc = Sched(nc, es)

    def sb(name, shape, dt):
        return es.enter_context(nc.sbuf_tensor(name, list(shape), dt))

    NPAGE = 38
    arena = sb("arena", [128, NPAGE * PAGE], BF16)

    def pages(p0, n):
        return arena[:, p0 * PAGE:(p0 + n) * PAGE]

    ident_bf = sb("ident_bf", [128, 128], BF16)
    ident_f = sb("ident_f", [128, 128], F32)
    tri_bf = sb("tri_bf", [128, 512], BF16)
    anti_bf = sb("anti_bf", [128, 512], BF16)
    cmpmask_bf = sb("cmpmask_bf", [128, S], BF16)
    selconst = sb("selconst", [128, NT, 32], F32)
    w2_bf = sb("w2_bf", [128, 2, 64], BF16)
    b1c = sb("b1c", [128, 2], F32)
    const1 = sb("const1", [128, 2], F32)
    b2k = sb("b2k", [64, 1], F32)
    b2v_bf = sb("b2v_bf", [1, 64], BF16)
    ones_bf = sb("ones_bf", [1, 128], BF16)
    peT_bf = sb("peT_bf", [64, 2, 32], BF16)
    convw = sb("convw", [128, 4, 3], F32)
    wr_f = sb("wr_f", [128, 8, 36], F32)
    wr_hi = sb("wr_hi", [128, 8, 36], BF16)
    wr_lo = sb("wr_lo", [128, 8, 36], BF16)
    br_b = sb("br_b", [128, 36], F32)
    pgb_bf = sb("pgb_bf", [1, D], BF16)
    vcaug = sb("vcaug", [128, 2, 97], BF16)
    kcT = sb("kcT", [64, 2, 128], BF16)
    negm = sb("negm", [128, 2, 96], BF16)
    PT = [sb(f"PT{i}", [128, 512], BF16) for i in range(4)]
    tmp = sb("tmp", [128, 12288], BF16)

    def tmpf(off, n):
        return tmp[:, 2 * off:2 * (off + n)].bitcast(F32)

    ps = [es.enter_context(nc.psum_tensor(f"ps{i}", [128, 512], F32)) for i in range(8)]
    psb = [sc.buf(f"ps{i}") for i in range(8)]

    ones128 = sb("ones128", [128, 128], BF16)
    lstrict = sb("lstrict", [128, 128], BF16)
    thr128 = sb("thr128", [128, 32], F32)
    sval = sb("sval", [128, 64], F32)
    pidx = sb("pidx", [128, 1], F32)
    OH_all = sb("OH_all", [128, NT, 3, 32], BF16)
    wk_all = sb("wk_all", [128, NT, 2], F32)
    posu = sb("posu", [128, NT, 2], mybir.dt.uint32)
    idxe = sb("idxe", [128, 64], mybir.dt.uint32)
    es_tiles = {"yit": [sb("yit0", [128, D], F32), sb("yit1", [128, D], F32)]}
    B_const = sc.buf("const")
    B_ysg = sc.buf("ysg")
    B_wbf = [sc.buf() for _ in range(32)]
    cq = Rot(["pool"])

    def pdma(out, in_, key, reads=(), writes=(), maxlast=None):
        if maxlast:
            sc.dma("pool", lambda e: e.dma_start(out=out, in_=in_, max_dma_last_dim=maxlast), key, reads, writes)
        else:
            sc.dma("pool", lambda e: e.dma_start(out=out, in_=in_), key, reads, writes)

    def sdma(out, in_, key, reads=(), writes=()):
        sc.dma("sp", lambda e: e.dma_start(out=out, in_=in_), key, reads, writes)

    def mm(specs):
        def fn(e):
            last = None
            for (o, l, r, st, sp) in specs:
                last = e.matmul(o, lhsT=l, rhs=r, start=st, stop=sp)
            return last
        return fn

    def PE(specs, reads, writes):
        sc.op("pe", mm(specs), reads, writes)

    def ACT(out, in_, func, reads, writes, **kw):
        sc.op("act", lambda e: e.activation(out=out, in_=in_, func=func, **kw), reads, writes)

    def DVE(fn, reads, writes):
        sc.op("dve", fn, reads, writes)

    def POOL(fn, reads, writes):
        sc.op("pool", fn, reads, writes)

    def dump(name, ap_sb, rd, idx=None):
        if name in dbg_d:
            dst = dbg_d[name] if idx is None else dbg_d[name][idx]
            sdma(dst, ap_sb, "dbg", reads=rd)

    K = "cst"
    pdma(ident_bf[:], c_ident[:, :], K, writes=[B_const])
    sdma(ident_f[:], c_ident[:, :], "cst2", writes=[B_const])
    pdma(tri_bf[:], c_tri[:, :], K, writes=[B_const])
    pdma(anti_bf[:], c_anti[:, :], K, writes=[B_const])
    pdma(cmpmask_bf[:], c_cmpmask[:, :], K, writes=[B_const])
    sdma(selconst[:].rearrange("p a b -> p (a b)"), c_selconst[:, :], "cst2", writes=[B_const])
    pdma(w2_bf[:], w2_d.rearrange("k h d -> h k d"), K, writes=[B_const])
    sdma(b1c[:], b1_d[:, :], "cst2", writes=[B_const])
    sdma(b2k[:], b2k_d[:, :], "cst2", writes=[B_const])
    pdma(b2v_bf[:], b2v_d[:, :], K, writes=[B_const])
    pdma(peT_bf[:], peT_d.rearrange("k d l -> d k l"), K, writes=[B_const])
    sdma(convw[:], convw_d.rearrange("c p k -> p c k"), "cst2", writes=[B_const])
    sdma(wr_f[:], wr_d.rearrange("(kc p) n -> p kc n", p=128), "cst2", writes=[B_const])
    sdma(br_b[:], br_d.partition_broadcast(128).rearrange("p o n -> p (o n)"), "cst2", writes=[B_const])
    pdma(pgb_bf[:], pgb_d[:, :], K, writes=[B_const])
    pdma(ones128[:], c_ones[:, :], K, writes=[B_const])
    pdma(lstrict[:], c_lstrict[:, :], K, writes=[B_const])
    sdma(thr128[:], c_thr128[:, :], "cst2", writes=[B_const])
    sdma(sval[:], c_sval[:, :], "cst2", writes=[B_const])
    sdma(pidx[:], c_pidx[:, :], "cst2", writes=[B_const])
    for g in range(2):
        pdma(vcaug[:, g, 65:97], c_overlap[:, :], K, writes=[B_const])
    POOL(lambda e: e.memset(ones_bf[:], 1.0), [], [B_const])
    POOL(lambda e: e.memset(vcaug[:, :, 64:65], 1.0), [], [B_const])
    POOL(lambda e: e.memset(negm[:], 0.0), [], [B_const])
    sc.barrier()
    DVE(lambda e: e.tensor_copy(out=wr_hi[:], in_=wr_f[:]), [B_const], [B_const])
    DVE(lambda e: e.tensor_tensor(out=wr_lo[:], in0=wr_f[:], in1=wr_hi[:], op=ALU.subtract), [B_const], [B_const])
    sc.barrier()

    for seq in range(NSEQ):
        xT = pages(0, 8).rearrange("p (k t) -> p k t", k=8)
        qT = pages(8, 8).rearrange("p (h t) -> p h t", h=8)
        kcmpT = pages(16, 2).rearrange("p (g t) -> p g t", g=2)
        vcmpT = pages(18, 2).rearrange("p (g t) -> p g t", g=2)
        kslcT = pages(20, 2).rearrange("p (g t) -> p g t", g=2)
        kwinT = pages(22, 2).rearrange("p (g t) -> p g t", g=2)
        misc = pages(24, 4)
        vslc = misc[:, 0:2080].rearrange("p (t g c) -> p t g c", t=NT, g=2)
        vwin = misc[:, 2080:4160].rearrange("p (t g c) -> p t g c", t=NT, g=2)
        gates = misc[:, 4160:4160 + 768].bitcast(F32).rearrange("p (t c) -> p t c", t=NT)
        impacc = misc[:, 4928:4928 + 2048].bitcast(F32).rearrange("p (t g m) -> p t g m", t=NT, g=2)
        o_tok = pages(28, 4).rearrange("p (t c) -> p t c", t=NT)
        wA = pages(28, 6)[:, 0:8 * NA].rearrange("p (k c) -> p k c", k=8)
        W1 = pages(34, 4).rearrange("p (k l h) -> p k l h", k=2, l=32)

        B_xT = [sc.buf() for _ in range(8)]
        B_wA = sc.buf()
        B_W1 = sc.buf()
        B_q = [[sc.buf() for _ in range(4)] for _ in range(8)]
        B_qm = [[sc.buf() for _ in range(4)] for _ in range(8)]
        B_kc = [sc.buf() for _ in range(2)]
        B_vc = [sc.buf() for _ in range(2)]
        B_ks = [[sc.buf() for _ in range(4)] for _ in range(2)]
        B_kw = [[sc.buf() for _ in range(4)] for _ in range(2)]
        B_ee = sc.buf()
        B_v = [sc.buf() for _ in range(NT)]
        B_g = [sc.buf() for _ in range(NT)]
        B_imp = [sc.buf() for _ in range(4)]
        B_o = [sc.buf() for _ in range(4)]
        B_PT = [sc.buf() for _ in range(4)]

        for kc in range(8):
            pdma(xT[:, kc, :], xT_d[seq, kc * 128:(kc + 1) * 128, :], f"xT{kc}", writes=[B_xT[kc]])
        pdma(wA, w_in_d.rearrange("(k p) c -> p k c", p=128)[:, :, 0:NA], "wA", writes=[B_wA])
        for k in range(2):
            pdma(W1[0:64, k], w1_d[k].rearrange("(l d) h -> d l h", d=64), "W1", writes=[B_W1])
        pdma(kslcT[64:96, 0, :], c_eexp[:, :], "ee", writes=[B_ee])
        pdma(kslcT[64:96, 1, :], c_eexp[:, :], "ee", writes=[B_ee])
        if seq == 0:
            for ex in range(32):
                tg = wgu_tab[ex * 128:(ex + 1) * 128, :].rearrange("p (k c) -> p k c", k=8)
                pdma(tg[:, :, 0:256], wg_d[ex].rearrange("(k p) c -> p k c", p=128), f"cv{ex}", writes=[B_wbf[ex]])
                pdma(tg[:, :, 256:512], wu_d[ex].rearrange("(k p) c -> p k c", p=128), f"cv{ex}", writes=[B_wbf[ex]])
                pdma(wd_tab[ex * 128:(ex + 1) * 128, :].rearrange("p (k c) -> p k c", k=2),
                     wd_d[ex].rearrange("(k p) c -> p k c", p=128), f"cv{ex}", writes=[B_wbf[ex]])
        DVE(lambda e: e.memset(vslc[:, :, :, 64:65], 1.0), [], B_v)
        DVE(lambda e: e.memset(vwin[:, :, :, 64:65], 1.0), [], B_v)

        rot = Rot(range(8))
        evq = Rot(["act", "dve"])

        def evac(out, in_, reads, writes):
            if evq.next() == "act":
                ACT(out, in_, AF.Copy, reads, writes)
            else:
                DVE(lambda e: e.tensor_copy(out=out, in_=in_), reads, writes)

        fm = []
        for h in range(8):
            fm.append((C_Q + 64 * h, (lambda c, h=h: qT[0:64, h, c * 512:(c + 1) * 512]), (lambda c, h=h: B_q[h][c])))
        for g in range(2):
            fm.append((C_KC + 64 * g, (lambda c, g=g: kcmpT[0:64, g, c * 512:(c + 1) * 512]), (lambda c, g=g: B_kc[g])))
            fm.append((C_VC + 64 * g, (lambda c, g=g: vcmpT[0:64, g, c * 512:(c + 1) * 512]), (lambda c, g=g: B_vc[g])))
            fm.append((C_KS + 64 * g, (lambda c, g=g: kslcT[0:64, g, c * 512:(c + 1) * 512]), (lambda c, g=g: B_ks[g][c])))
            fm.append((C_KW + 64 * g, (lambda c, g=g: kwinT[0:64, g, c * 512:(c + 1) * 512]), (lambda c, g=g: B_kw[g][c])))
        for (c0, dst, dbuf) in fm:
            for c in range(4):
                b = rot.next()
                PE([(ps[b][0:64, :], wA[:, kc, c0:c0 + 64], xT[:, kc, c * 512:(c + 1) * 512], kc == 0, kc == 7)
                    for kc in range(8)], B_xT + [B_wA], [psb[b]])
                evac(dst(c), ps[b][0:64, :], [psb[b]], [dbuf(c)])
        for t in range(NT):
            b = rot.next()
            PE([(ps[b][:, 0:408], xT[:, kc, t * 128:(t + 1) * 128], wA[:, kc, C_VS:C_VS + 408], kc == 0, kc == 7)
                for kc in range(8)], B_xT + [B_wA], [psb[b]])
            ACT(vslc[:, t, :, 0:64], ps[b][:, 0:128].rearrange("p (g c) -> p g c", g=2), AF.Copy, [psb[b]], [B_v[t]])
            DVE(lambda e, b=b, t=t: e.tensor_copy(out=vwin[:, t, :, 0:64],
                                                  in_=ps[b][:, 256:384].rearrange("p (g c) -> p g c", g=2)),
                [psb[b]], [B_v[t]])
            ACT(gates[:, t, :], ps[b][:, 384:408], AF.Sigmoid, [psb[b]], [B_g[t]])
        if dbg and "qT" in dbg_d:
            for h in range(8):
                tq = tmpf(0, 2048)
                Bt = sc.buf()
                DVE(lambda e, h=h: e.tensor_copy(out=tq[0:64, :], in_=qT[0:64, h, :]), B_q[h], [Bt])
                sdma(dbg_d["qT"][h], tq[0:64, :], "dbg", reads=[Bt])
                sc.barrier()
        sc.barrier()
        if stop_after == 1:
            continue

        if seq == 0:
            for k in range(2):
                b = rot.next()
                PE([(ps[b][:, 0:1], W1[0:64, k, l, :], peT_bf[:, k, l:l + 1], l == 0, l == 31) for l in range(32)],
                   [B_W1, B_const], [psb[b]])
                DVE(lambda e, b=b, k=k: e.tensor_tensor(out=const1[:, k:k + 1], in0=ps[b][:, 0:1], in1=b1c[:, k:k + 1], op=ALU.add),
                    [psb[b], B_const], [B_const])
        B_t = [sc.buf() for _ in range(6)]
        cx = tmpf(0, 128)
        ct2 = tmpf(128, 128)
        csg = tmpf(256, 128)
        hT = tmp[:, 1024:1024 + 128]
        for k in range(2):
            src, Bsrc = (kcmpT, B_kc) if k == 0 else (vcmpT, B_vc)
            for g in range(2):
                b = rot.next()
                PE([(ps[b][:, 0:127], W1[0:64, k, l, :], src[0:64, g, l:l + 16 * 126 + 1:16], l == 0, l == 31)
                    for l in range(32)], [B_W1, Bsrc[g]], [psb[b]])
                DVE(lambda e, b=b, k=k: e.tensor_scalar(out=cx[:, 0:127], in0=ps[b][:, 0:127], scalar1=const1[:, k:k + 1],
                                                        scalar2=None, op0=ALU.add), [psb[b], B_const], [B_t[0]])
                DVE(lambda e: e.tensor_tensor(out=ct2[:, 0:127], in0=cx[:, 0:127], in1=cx[:, 0:127], op=ALU.mult),
                    [B_t[0]], [B_t[1]])
                DVE(lambda e: e.tensor_scalar(out=ct2[:, 0:127], in0=ct2[:, 0:127], scalar1=0.044715, scalar2=1.0,
                                              op0=ALU.mult, op1=ALU.add), [B_t[1]], [B_t[1]])
                DVE(lambda e: e.tensor_tensor(out=ct2[:, 0:127], in0=ct2[:, 0:127], in1=cx[:, 0:127], op=ALU.mult),
                    [B_t[0], B_t[1]], [B_t[1]])
                ACT(csg[:, 0:127], ct2[:, 0:127], AF.Sigmoid, [B_t[1]], [B_t[2]], scale=1.5957691216057308)
                DVE(lambda e: e.tensor_tensor(out=hT[:, 0:127], in0=cx[:, 0:127], in1=csg[:, 0:127], op=ALU.mult),
                    [B_t[0], B_t[2]], [B_t[3]])
                b2 = rot.next()
                if k == 0:
                    PE([(ps[b2][0:64, 0:127], w2_bf[:, 0, :], hT[:, 0:127], True, True)], [B_t[3], B_const], [psb[b2]])
                    DVE(lambda e, b2=b2, g=g: e.tensor_scalar(out=kcT[:, g, 0:127], in0=ps[b2][0:64, 0:127], scalar1=b2k[:, 0:1],
                                                              scalar2=None, op0=ALU.add), [psb[b2], B_const], [B_kc[g]])
                else:
                    PE([(ps[b2][0:127, 0:64], hT[:, 0:127], w2_bf[:, 1, :], True, False),
                        (ps[b2][0:127, 0:64], ones_bf[0:1, 0:127], b2v_bf[0:1, :], False, True)],
                       [B_t[3], B_const], [psb[b2]])
                    DVE(lambda e, b2=b2, g=g: e.tensor_copy(out=vcaug[0:127, g, 0:64], in_=ps[b2][0:127, 0:64]),
                        [psb[b2]], [B_vc[g]])
        sc.barrier()

        sbank = Rot([0, 1, 2, 3])
        abank = Rot([4, 5])
        ptrot = Rot(range(4))
        rs = tmpf(0, 4)
        sgate = tmpf(8, 4)
        otmp = tmpf(16, 256)
        itmp = tmpf(272, 128)
        B_rs, B_sg, B_ot, B_it = sc.buf(), sc.buf(), sc.buf(), sc.buf()

        def evac_attn(ab, hh, c, br, ncol, first):
            acc = ps[ab][:, :].rearrange("p (j c) -> p j c", j=4)
            g = hh // 4
            DVE(lambda e: e.tensor_scalar(out=rs[:, 0:4], in0=acc[:, :, 64:65].rearrange("p j o -> p (j o)"), scalar1=1e-30,
                                          scalar2=None, op0=ALU.add), [psb[ab]], [B_rs])
            DVE(lambda e: e.reciprocal(out=rs[:, 0:4], in_=rs[:, 0:4]), [B_rs], [B_rs])
            DVE(lambda e: e.tensor_tensor(out=sgate[:, 0:4], in0=rs[:, 0:4], in1=gates[:, 4 * c:4 * c + 4, hh * 3 + br],
                                          op=ALU.mult), [B_rs] + B_g[4 * c:4 * c + 4], [B_sg])
            dst = o_tok[:, 4 * c:4 * c + 4, hh * 64:(hh + 1) * 64]
            sgb = sgate[:, 0:4].unsqueeze(2).to_broadcast([128, 4, 64])
            if first:
                DVE(lambda e: e.tensor_tensor(out=dst, in0=acc[:, :, 0:64], in1=sgb, op=ALU.mult),
                    [psb[ab], B_sg], [B_o[c]])
            else:
                ot = otmp[:, 0:256].rearrange("p (j c) -> p j c", j=4)
                DVE(lambda e: e.tensor_tensor(out=ot, in0=acc[:, :, 0:64], in1=sgb, op=ALU.mult),
                    [psb[ab], B_sg], [B_ot])
                DVE(lambda e: e.tensor_tensor(out=dst, in0=dst, in1=ot, op=ALU.add), [B_ot, B_o[c]], [B_o[c]])
            if br == 0:
                rsb = rs[:, 0:4].unsqueeze(2).to_broadcast([128, 4, 32])
                idst = impacc[:, 4 * c:4 * c + 4, g, :]
                if hh % 4 == 0:
                    DVE(lambda e: e.tensor_tensor(out=idst, in0=acc[:, :, 65:97], in1=rsb, op=ALU.mult),
                        [psb[ab], B_rs], [B_imp[c]])
                else:
                    it = itmp[:, 0:128].rearrange("p (j c) -> p j c", j=4)
                    DVE(lambda e: e.tensor_tensor(out=it, in0=acc[:, :, 65:97], in1=rsb, op=ALU.mult),
                        [psb[ab], B_rs], [B_it])
                    DVE(lambda e: e.tensor_tensor(out=idst, in0=idst, in1=it, op=ALU.add), [B_it, B_imp[c]], [B_imp[c]])

        for hh in range(8):
            g = hh // 4
            for c in range(4):
                b = sbank.next()
                PE([(ps[b][0:127, :], kcT[:, g, 0:127], qT[0:64, hh, c * 512:(c + 1) * 512], True, False),
                    (ps[b][0:127, :], ident_bf[0:127, 0:127], cmpmask_bf[0:127, c * 512:(c + 1) * 512], False, True)],
                   [B_kc[g], B_q[hh][c], B_const], [psb[b]])
                pi = ptrot.next()
                ACT(PT[pi][0:127, :], ps[b][0:127, :], AF.Exp, [psb[b]], [B_PT[pi]], scale=0.125)
                ab = abank.next()
                PE([(ps[ab][:, j * 128:j * 128 + 97], PT[pi][0:127, j * 128:(j + 1) * 128], vcaug[0:127, g, :], True, True)
                    for j in range(4)], [B_PT[pi], B_vc[g], B_const], [psb[ab]])
                evac_attn(ab, hh, c, 0, 128, True)
        score = tmpf(512, 32)
        work = tmpf(544, 32)
        m8a = tmpf(576, 8)
        m8b = tmpf(584, 8)
        msk = tmpf(592, 32)
        B_s = [sc.buf() for _ in range(5)]
        B_negm = sc.buf()
        for c in range(4):
            for g in range(2):
                b = sbank.next()
                for j in range(4):
                    t = 4 * c + j
                    DVE(lambda e, t=t, g=g: e.tensor_tensor(out=score[:, :], in0=impacc[:, t, g, :], in1=selconst[:, t, :], op=ALU.add),
                        [B_imp[c], B_const], [B_s[0]])
                    DVE(lambda e: e.max(out=m8a[:, :], in_=score[:, :]), [B_s[0]], [B_s[1]])
                    DVE(lambda e: e.match_replace(out=work[:, :], in_to_replace=m8a[:, :], in_values=score[:, :], imm_value=-1e9),
                        [B_s[0], B_s[1]], [B_s[2]])
                    DVE(lambda e: e.max(out=m8b[:, :], in_=work[:, :]), [B_s[2]], [B_s[3]])
                    DVE(lambda e: e.tensor_scalar(out=msk[:, :], in0=score[:, :], scalar1=m8b[:, 7:8], scalar2=None, op0=ALU.is_ge),
                        [B_s[0], B_s[3]], [B_s[4]])
                    DVE(lambda e, g=g: e.tensor_scalar(out=negm[:, g, 64:96], in0=msk[:, :], scalar1=-1.0, scalar2=-NEG,
                                                       op0=ALU.add, op1=ALU.mult), [B_s[4]], [B_negm])
                    PE([(ps[b][0:96, j * 128:(j + 1) * 128], negm[:, g, :], ident_bf[:, :], True, True)],
                       [B_negm, B_const], [psb[b]])
                    if "msk" in dbg_d:
                        sdma(dbg_d["msk"][g, t], msk[:, :], "dbg", reads=[B_s[4]])
                        sc.barrier()
                DVE(lambda e, b=b, g=g, c=c: e.tensor_copy(
                    out=qT[64:96, 4 * g:4 * g + 4, c * 512:(c + 1) * 512],
                    in_=ps[b][64:96, :].unsqueeze(1).to_broadcast([32, 4, 512])),
                    [psb[b]], [B_qm[4 * g + h][c] for h in range(4)])
        sc.barrier()

        def attn_branch(br, kT, Bk, vtok, krows):
            pending = []

            def flush(n):
                while len(pending) > n:
                    pending.pop(0)()
            for hh in range(8):
                g = hh // 4
                for c in range(4):
                    ab = abank.next()
                    kt0 = 0 if br == 1 else max(0, 4 * c - 4)
                    kts = list(range(kt0, 4 * c + 4))
                    for kt in kts:
                        r = kt - 4 * c
                        if r >= 0:
                            j0, j1 = r, 4
                        elif br == 1:
                            j0, j1 = 0, 4
                        else:
                            j0, j1 = 0, r + 5
                        q0, q1 = c * 512 + j0 * 128, c * 512 + j1 * 128
                        N = q1 - q0
                        b = sbank.next()
                        specs = []
                        rd = [Bk[g][kt // 4], B_q[hh][c], B_const]
                        if br == 1:
                            rd += [B_qm[hh][c], B_ee]
                        if r >= 0:
                            specs.append((ps[b][:, 0:N], ident_bf[:, :], tri_bf[:, 0:N], True, False))
                        elif br == 2:
                            specs.append((ps[b][:, 0:N], ident_bf[:, :], anti_bf[:, 512 - N:512], True, False))
                        specs.append((ps[b][:, 0:N], kT[0:krows, g, kt * 128:(kt + 1) * 128], qT[0:krows, hh, q0:q1],
                                      len(specs) == 0, True))
                        PE(specs, rd, [psb[b]])
                        pi = ptrot.next()
                        ACT(PT[pi][:, 0:N], ps[b][:, 0:N], AF.Exp, [psb[b]], [B_PT[pi]], scale=0.125)

                        def att_s2(hh=hh, g=g, c=c, ab=ab, kt=kt, kts=kts, j0=j0, j1=j1, pi=pi):
                            pv = []
                            for j in range(j0, j1):
                                last_kt = 4 * c + j
                                pv.append((ps[ab][:, j * 128:j * 128 + 65], PT[pi][:, (j - j0) * 128:(j - j0 + 1) * 128],
                                           vtok[:, kt, g, :], (kt == kts[0] and j == j0), kt == last_kt))
                            PE(pv, [B_PT[pi], B_v[kt]], [psb[ab]])
                            if kt == kts[-1]:
                                evac_attn(ab, hh, c, br, 128, False)
                        pending.append(att_s2)
                        flush(2)
            flush(0)

        attn_branch(1, kslcT, B_ks, vslc, 96)
        attn_branch(2, kwinT, B_kw, vwin, 64)
        sc.barrier()
        if "o_tok" in dbg_d:
            for t in range(NT):
                tq = tmpf(0, 512)
                DVE(lambda e, t=t: e.tensor_copy(out=tq[:, :], in_=o_tok[:, t, :]), [], [])
                sc.barrier()
                sdma(dbg_d["o_tok"][t * 128:(t + 1) * 128, :], tq[:, :], "dbg")
                sc.barrier()
        if stop_after == 2:
            continue

        oT = pages(8, 4).rearrange("p (i t) -> p i t", i=4)
        B_oT = [sc.buf() for _ in range(4)]
        for c in range(4):
            for i in range(4):
                b = rot.next()
                pb = ps[b][:, :].bitcast(BF16)
                sc.op("pe", (lambda b=b, c=c, i=i, pb=pb: (lambda e: [e.transpose(pb[:, j * 128:(j + 1) * 128], o_tok[:, 4 * c + j, i * 128:(i + 1) * 128], ident_bf[:, :]) for j in range(4)][-1]))(),
                      [B_o[c], B_const], [psb[b]])
                evac(oT[:, i, c * 512:(c + 1) * 512], pb[:, 0:512], [psb[b]], [B_oT[c]])
        sc.barrier()

        buT = pages(12, 4).rearrange("p (i t) -> p i t", i=4)
        mergedT = pages(16, 8).rearrange("p (k t) -> p k t", k=8)
        wcB = pages(24, 6).rearrange("p (k c) -> p k c", k=8)
        wgm = [pages(30, 2).rearrange("p (k c) -> p k c", k=8), pages(32, 2).rearrange("p (k c) -> p k c", k=8)]
        wnsa = pages(34, 2).rearrange("p (k c) -> p k c", k=4)
        wcv = pages(36, 2).rearrange("p (k c) -> p k c", k=4)
        B_wcB, B_wnsa, B_wcv = sc.buf(), sc.buf(), sc.buf()
        B_wgm = [sc.buf(), sc.buf()]
        B_bu = [sc.buf() for _ in range(4)]
        B_mg = [sc.buf() for _ in range(4)]
        w_in_v = w_in_d.rearrange("(k p) c -> p k c", p=128)
        pdma(wcB, w_in_v[:, :, C_CB:C_CB + 1536], "wcB", writes=[B_wcB])
        pdma(wnsa, wnsa_d.rearrange("(k p) c -> p k c", p=128), "wnsa", writes=[B_wnsa])
        pdma(wcv, wcv_d.rearrange("(k p) c -> p k c", p=128), "wcv", writes=[B_wcv])
        c_sb = tmpf(0, 512)
        u = tmpf(512, 514)
        uc = tmpf(1026, 512)
        B_c, B_u, B_uc = sc.buf(), sc.buf(), sc.buf()
        for cb in range(4):
            for c in range(4):
                bb, bc, bh = rot.next(), rot.next(), rot.next()
                for (bk, off) in ((bb, 0), (bc, 512), (bh, 1024)):
                    PE([(ps[bk][:, :], wcB[:, kc, off + cb * 128:off + (cb + 1) * 128], xT[:, kc, c * 512:(c + 1) * 512], kc == 0, kc == 7)
                        for kc in range(8)], B_xT + [B_wcB], [psb[bk]])
                if c == 0:
                    DVE(lambda e: e.memset(u[:, 0:2], 0.0), [B_u], [B_u])
                else:
                    DVE(lambda e: e.tensor_copy(out=u[:, 0:2], in_=u[:, 512:514]), [B_u], [B_u])
                ACT(c_sb[:, :], ps[bc][:, :], AF.Copy, [psb[bc]], [B_c])
                DVE(lambda e, bh=bh: e.tensor_tensor(out=u[:, 2:514], in0=c_sb[:, :], in1=ps[bh][:, :], op=ALU.mult),
                    [B_c, psb[bh]], [B_u])
                DVE(lambda e, cb=cb: e.tensor_scalar(out=uc[:, :], in0=u[:, 0:512], scalar1=convw[:, cb, 0:1], scalar2=None, op0=ALU.mult),
                    [B_u, B_const], [B_uc])
                DVE(lambda e, cb=cb: e.scalar_tensor_tensor(out=uc[:, :], in0=u[:, 1:513], scalar=convw[:, cb, 1:2], in1=uc[:, :],
                                                            op0=ALU.mult, op1=ALU.add), [B_u, B_uc], [B_uc])
                DVE(lambda e, cb=cb: e.scalar_tensor_tensor(out=uc[:, :], in0=u[:, 2:514], scalar=convw[:, cb, 2:3], in1=uc[:, :],
                                                            op0=ALU.mult, op1=ALU.add), [B_u, B_uc], [B_uc])
                DVE(lambda e, bb=bb, cb=cb, c=c: e.tensor_tensor(out=buT[:, cb, c * 512:(c + 1) * 512], in0=uc[:, :], in1=ps[bb][:, :], op=ALU.mult),
                    [B_uc, psb[bb]], [B_bu[c]])
        s1 = tmpf(0, 512)
        s2 = tmpf(512, 512)
        m1 = tmpf(1024, 512)
        m2 = tmpf(1536, 512)
        B_s1, B_s2, B_m1, B_m2 = sc.buf(), sc.buf(), sc.buf(), sc.buf()
        for dc in range(8):
            wq = wgm[dc % 2]
            pdma(wq[:, :, 0:128], w_in_v[:, :, C_G1 + dc * 128:C_G1 + (dc + 1) * 128], f"wgm{dc % 2}", writes=[B_wgm[dc % 2]])
            pdma(wq[:, :, 128:256], w_in_v[:, :, C_G2 + dc * 128:C_G2 + (dc + 1) * 128], f"wgm{dc % 2}", writes=[B_wgm[dc % 2]])
            for c in range(4):
                b1, b2, b3, b4 = rot.next(), rot.next(), rot.next(), rot.next()
                tok = slice(c * 512, (c + 1) * 512)
                PE([(ps[b1][:, :], wq[:, kc, 0:128], xT[:, kc, tok], kc == 0, kc == 7) for kc in range(8)],
                   B_xT + [B_wgm[dc % 2]], [psb[b1]])
                PE([(ps[b2][:, :], wq[:, kc, 128:256], xT[:, kc, tok], kc == 0, kc == 7) for kc in range(8)],
                   B_xT + [B_wgm[dc % 2]], [psb[b2]])
                PE([(ps[b3][:, :], wnsa[:, i, dc * 128:(dc + 1) * 128], oT[:, i, tok], i == 0, i == 3) for i in range(4)],
                   [B_wnsa, B_oT[c]], [psb[b3]])
                PE([(ps[b4][:, :], wcv[:, i, dc * 128:(dc + 1) * 128], buT[:, i, tok], i == 0, i == 3) for i in range(4)],
                   [B_wcv, B_bu[c]], [psb[b4]])
                ACT(s1[:, :], ps[b1][:, :], AF.Sigmoid, [psb[b1]], [B_s1])
                ACT(s2[:, :], ps[b2][:, :], AF.Sigmoid, [psb[b2]], [B_s2])
                DVE(lambda e, b3=b3: e.tensor_tensor(out=m1[:, :], in0=s1[:, :], in1=ps[b3][:, :], op=ALU.mult),
                    [B_s1, psb[b3]], [B_m1])
                DVE(lambda e, b4=b4: e.tensor_tensor(out=m2[:, :], in0=s2[:, :], in1=ps[b4][:, :], op=ALU.mult),
                    [B_s2, psb[b4]], [B_m2])
                DVE(lambda e, dc=dc, tok=tok: e.tensor_tensor(out=mergedT[:, dc, tok], in0=m1[:, :], in1=m2[:, :], op=ALU.add),
                    [B_m1, B_m2], [B_mg[c]])
        sc.barrier()
        if stop_after == 3:
            continue

        pT_bf = pages(13, 2).rearrange("p (k t) -> p k t", k=2)
        B_pT = sc.buf()
        pdma(pT_bf, pT_d[seq].rearrange("(k p) t -> p k t", p=128), "pT", writes=[B_pT])
        lnt = pages(9, 4).bitcast(F32).rearrange("p (a d) -> p a d", a=4)
        B_ln = sc.buf()
        for a, src in enumerate((ln1g_d, ln1b_d, ln2g_d, ln2b_d)):
            sdma(lnt[:, a, :], src.partition_broadcast(128).rearrange("p o d -> p (o d)"), "ln", writes=[B_ln])
        pproj = pages(8, 1).rearrange("p (k c) -> p k c", k=2)
        B_pproj = sc.buf()
        pdma(pproj, pproj_d.rearrange("(k p) c -> p k c", p=128), "pproj", writes=[B_pproj])
        wo = pages(0, 4).rearrange("p (k c) -> p k c", k=8)
        pgw = pages(4, 4).rearrange("p (k c) -> p k c", k=8)
        B_wo, B_pgw = sc.buf(), sc.buf()
        pdma(wo, wo_d.rearrange("(k p) c -> p k c", p=128), "wo", writes=[B_wo])
        pdma(pgw, pgw_d.rearrange("(k p) c -> p k c", p=128), "pgw", writes=[B_pgw])
        x_hi_all = pages(24, 8).rearrange("p (t d) -> p t d", t=NT)
        B_xhi = [sc.buf() for _ in range(NT)]
        B_oh = [sc.buf() for _ in range(NT)]
        B_wk = [sc.buf() for _ in range(NT)]
        B_yi = [sc.buf() for _ in range(NT)]
        xt = [tmpf(0, 1024), tmpf(1024, 1024)]
        B_xt = [sc.buf(), sc.buf()]
        h1 = tmpf(2048, 1024)
        x_lo = tmp[:, 6144:7168]
        x1Tt = tmp[:, 7168:8192].rearrange("p (k t) -> p k t", k=8)
        sgt = tmpf(4096, 512)
        st6 = tmpf(4608, 16)
        mv = tmpf(4624, 4)
        rt = tmpf(4640, 96)
        x1Tlo = tmp[:, 10240:11264].rearrange("p (k t) -> p k t", k=8)
        yin = [tmpf(4736, 384), None]
        yi_t = tmp[:, 11264:12288]
        B_h1, B_x1Tf, B_sgt, B_st, B_mv, B_xlo, B_x1Tt = (sc.buf() for _ in range(7))
        B_rt = [sc.buf() for _ in range(14)]
        yit = es_tiles["yit"]
        B_yit = [sc.buf(), sc.buf()]

        def layernorm(src, Bsrc, dst, Bdst, ga, ba):
            for hf in range(2):
                DVE(lambda e, hf=hf: e.bn_stats(out=st6[:, hf * 6:(hf + 1) * 6], in_=src[:, hf * 512:(hf + 1) * 512]),
                    [Bsrc], [B_st])
            DVE(lambda e: e.bn_aggr(out=mv[:, 0:2], in_=st6[:, 0:12]), [B_st], [B_mv])
            DVE(lambda e: e.tensor_scalar(out=mv[:, 1:2], in0=mv[:, 1:2], scalar1=EPS, scalar2=None, op0=ALU.add),
                [B_mv], [B_mv])
            ACT(mv[:, 1:2], mv[:, 1:2], AF.Sqrt, [B_mv], [B_mv])
            DVE(lambda e: e.reciprocal(out=mv[:, 1:2], in_=mv[:, 1:2]), [B_mv], [B_mv])
            DVE(lambda e: e.tensor_scalar(out=dst, in0=src, scalar1=mv[:, 0:1], scalar2=mv[:, 1:2], op0=ALU.subtract, op1=ALU.mult),
                [Bsrc, B_mv], [Bdst])
            DVE(lambda e: e.tensor_tensor(out=dst, in0=dst, in1=lnt[:, ga, :], op=ALU.mult), [Bdst, B_ln], [Bdst])
            DVE(lambda e: e.tensor_tensor(out=dst, in0=dst, in1=lnt[:, ba, :], op=ALU.add), [Bdst, B_ln], [Bdst])

        for t in range(NT):
            tsl = slice(t * 128, (t + 1) * 128)
            xb = t % 2
            sdma(xt[xb][:, :], x_d[seq, tsl, :], f"xt{xb}", writes=[B_xt[xb]])
            bm = (rot.next(), rot.next())
            for hf in range(2):
                PE([(ps[bm[hf]][:, :], mergedT[:, dc, tsl], wo[:, dc, hf * 512:(hf + 1) * 512], dc == 0, dc == 7) for dc in range(8)],
                   [B_mg[t // 4], B_wo], [psb[bm[hf]]])
                DVE(lambda e, hf=hf, b=bm[hf], xb=xb: e.scalar_tensor_tensor(
                    out=h1[:, hf * 512:(hf + 1) * 512], in0=xt[xb][:, hf * 512:(hf + 1) * 512], scalar=ALPHA,
                    in1=ps[b][:, :], op0=ALU.mult, op1=ALU.add), [B_xt[xb], psb[bm[hf]]], [B_h1])
            layernorm(h1[:, :], B_h1, h1[:, :], B_h1, 0, 1)
            x_hi = x_hi_all[:, t, :]
            DVE(lambda e, x_hi=x_hi: e.tensor_copy(out=x_hi, in_=h1[:, :]), [B_h1], [B_xhi[t]])
            DVE(lambda e, x_hi=x_hi: e.tensor_tensor(out=x_lo[:, :], in0=h1[:, :], in1=x_hi, op=ALU.subtract), [B_h1, B_xhi[t]], [B_xlo])
            bt = (rot.next(), rot.next())
            for which in range(2):
                srcx, Bsrcx = (x_hi, B_xhi[t]) if which == 0 else (x_lo, B_xlo)
                b = bt[which]
                pb = ps[b][:, :].bitcast(BF16)
                sc.op("pe", (lambda pb=pb, srcx=srcx: (lambda e: [e.transpose(pb[:, j * 128:(j + 1) * 128], srcx[:, j * 128:(j + 1) * 128], ident_bf[:, :]) for j in range(8)][-1]))(),
                      [Bsrcx, B_const], [psb[b]])
                if which == 0:
                    ACT(x1Tt[:, :, :], pb[:, :].rearrange("p (k t) -> p k t", k=8), AF.Copy, [psb[b]], [B_x1Tt])
                else:
                    DVE(lambda e, pb=pb: e.tensor_copy(out=x1Tlo[:, :, :], in_=pb[:, :].rearrange("p (k t) -> p k t", k=8)), [psb[b]], [B_x1Tf])
            br_ = rot.next()
            specs = []
            for kc in range(8):
                specs.append((ps[br_][:, 0:36], x1Tt[:, kc, :], wr_hi[:, kc, :], kc == 0, False))
                specs.append((ps[br_][:, 0:36], x1Tlo[:, kc, :], wr_hi[:, kc, :], False, False))
                specs.append((ps[br_][:, 0:36], x1Tt[:, kc, :], wr_lo[:, kc, :], False, kc == 7))
            PE(specs, [B_x1Tf, B_x1Tt, B_const], [psb[br_]])
            lg = rt[:, 0:36]
            gmax, gsum, ch, m8, wv, mk1, mk2 = (rt[:, 36:37], rt[:, 37:38], rt[:, 44:52], rt[:, 52:60], rt[:, 60:62], rt[:, 64:72], rt[:, 72:80])
            gex = rt[:, 88:92]
            ohu = rt[:, 92:96]
            DVE(lambda e, b=br_: e.tensor_tensor(out=lg, in0=ps[b][:, 0:36], in1=br_b[:, :], op=ALU.add), [psb[br_], B_const], [B_rt[0]])
            DVE(lambda e: e.tensor_reduce(out=gmax, in_=lg[:, 0:4], axis=AX.X, op=ALU.max), [B_rt[0]], [B_rt[1]])
            DVE(lambda e: e.tensor_scalar(out=gex, in0=lg[:, 0:4], scalar1=gmax, scalar2=None, op0=ALU.subtract), [B_rt[0], B_rt[1]], [B_rt[3]])
            ACT(gex, gex, AF.Exp, [B_rt[3]], [B_rt[3]])
            DVE(lambda e: e.tensor_reduce(out=gsum, in_=gex, axis=AX.X, op=ALU.add), [B_rt[3]], [B_rt[4]])
            DVE(lambda e: e.reciprocal(out=gsum, in_=gsum), [B_rt[4]], [B_rt[4]])
            DVE(lambda e: e.tensor_scalar(out=ohu, in0=lg[:, 0:4], scalar1=gmax, scalar2=None, op0=ALU.is_ge), [B_rt[0], B_rt[1]], [B_rt[5]])
            DVE(lambda e: e.tensor_scalar(out=ch, in0=lg[:, 4:12], scalar1=ohu[:, 0:1], scalar2=None, op0=ALU.mult), [B_rt[0], B_rt[5]], [B_rt[6]])
            for g in range(1, 4):
                DVE(lambda e, g=g: e.scalar_tensor_tensor(out=ch, in0=lg[:, 4 + 8 * g:12 + 8 * g], scalar=ohu[:, g:g + 1], in1=ch,
                                                          op0=ALU.mult, op1=ALU.add), [B_rt[0], B_rt[5], B_rt[6]], [B_rt[6]])
            DVE(lambda e: e.max(out=m8, in_=ch), [B_rt[6]], [B_rt[7]])
            DVE(lambda e: e.tensor_tensor(out=wv[:, 0:1], in0=m8[:, 0:1], in1=m8[:, 1:2], op=ALU.subtract), [B_rt[7]], [B_rt[8]])
            ACT(wv[:, 0:1], wv[:, 0:1], AF.Sigmoid, [B_rt[8]], [B_rt[8]])
            DVE(lambda e: e.tensor_scalar(out=wv[:, 1:2], in0=wv[:, 0:1], scalar1=-1.0, scalar2=1.0, op0=ALU.mult, op1=ALU.add), [B_rt[8]], [B_rt[8]])
            DVE(lambda e, t=t: e.tensor_scalar(out=wk_all[:, t, :], in0=wv[:, 0:2], scalar1=gsum, scalar2=None, op0=ALU.mult),
                [B_rt[8], B_rt[4]], [B_wk[t]])
            DVE(lambda e: e.tensor_scalar(out=mk1, in0=ch, scalar1=m8[:, 0:1], scalar2=None, op0=ALU.is_equal), [B_rt[6], B_rt[7]], [B_rt[9]])
            DVE(lambda e: e.tensor_scalar(out=mk2, in0=ch, scalar1=m8[:, 1:2], scalar2=None, op0=ALU.is_equal), [B_rt[6], B_rt[7]], [B_rt[10]])
            for k, (mk, Bmk) in enumerate(((mk1, B_rt[9]), (mk2, B_rt[10]))):
                DVE(lambda e, t=t, k=k, mk=mk: e.tensor_tensor(out=OH_all[:, t, k, :].rearrange("p (g x) -> p g x", g=4),
                                                               in0=ohu.unsqueeze(2).to_broadcast([128, 4, 8]),
                                                               in1=mk.unsqueeze(1).to_broadcast([128, 4, 8]), op=ALU.mult),
                    [B_rt[5], Bmk], [B_oh[t]])
            DVE(lambda e, t=t: e.tensor_tensor(out=OH_all[:, t, 2, :], in0=OH_all[:, t, 0, :], in1=OH_all[:, t, 1, :], op=ALU.add),
                [B_oh[t]], [B_oh[t]])
            yb_ = t % 2
            for hf in range(2):
                bg, bp = rot.next(), rot.next()
                cs = slice(hf * 512, (hf + 1) * 512)
                PE([(ps[bg][:, :], x1Tt[:, kc, :], pgw[:, kc, cs], kc == 0, False) for kc in range(8)]
                   + [(ps[bg][:, :], ones_bf[0:1, :], pgb_bf[0:1, cs], False, True)],
                   [B_x1Tt, B_pgw, B_const], [psb[bg]])
                PE([(ps[bp][:, :], pT_bf[:, k2, tsl], pproj[:, k2, cs], k2 == 0, k2 == 1) for k2 in range(2)],
                   [B_pT, B_pproj], [psb[bp]])
                ACT(sgt[:, :], ps[bg][:, :], AF.Sigmoid, [psb[bg]], [B_sgt])
                DVE(lambda e, bp=bp: e.tensor_tensor(out=sgt[:, :], in0=sgt[:, :], in1=ps[bp][:, :], op=ALU.mult), [B_sgt, psb[bp]], [B_sgt])
                DVE(lambda e, cs=cs, yb_=yb_: e.scalar_tensor_tensor(out=yit[yb_][:, cs], in0=h1[:, cs], scalar=ALPHA, in1=sgt[:, :],
                                                                      op0=ALU.mult, op1=ALU.add), [B_h1, B_sgt], [B_yit[yb_]])
            sdma(yinit_d[tsl, :], yit[yb_][:, :], f"yi{yb_}", reads=[B_yit[yb_]], writes=[B_yi[t]])
        sc.barrier()

        bc, brk = rot.next(), rot.next()
        PE([(ps[bc][:, 0:32], ones128[:, :], OH_all[:, t, 2, :], t == 0, t == NT - 1) for t in range(NT)], B_oh + [B_const], [psb[bc]])
        for t in range(NT):
            PE([(ps[brk][:, t * 32:(t + 1) * 32], lstrict[:, :], OH_all[:, t, 2, :], True, t == 0)]
               + [(ps[brk][:, t * 32:(t + 1) * 32], ones128[:, :], OH_all[:, t2, 2, :], False, t2 == t - 1) for t2 in range(t)],
               B_oh + [B_const], [psb[brk]])
        cnt = tmpf(0, 32)
        cmpm = tmpf(32, 1024).rearrange("p (e m) -> p e m", e=32)
        ntl = tmpf(1056, 32)
        cca = tmpf(1088, 32)
        ccb = tmpf(1120, 32)
        base = tmpf(1152, 32)
        val = tmpf(1184, 512).rearrange("p (t e) -> p t e", t=NT)
        prod = tmpf(1696, 512).rearrange("p (t e) -> p t e", t=NT)
        posf = tmpf(2208, 32).rearrange("p (t k) -> p t k", t=NT)
        cmp2 = tmpf(2240, 2048).rearrange("p (s e) -> p s e", s=64)
        esl = tmpf(4288, 64)
        Bq = [sc.buf() for _ in range(12)]
        DVE(lambda e, bc=bc: e.tensor_copy(out=cnt, in_=ps[bc][:, 0:32]), [psb[bc]], [Bq[0]])
        DVE(lambda e: e.tensor_tensor(out=cmpm, in0=cnt.unsqueeze(2).to_broadcast([128, 32, 32]),
                                      in1=thr128[:, :].unsqueeze(1).to_broadcast([128, 32, 32]), op=ALU.is_gt), [Bq[0], B_const], [Bq[1]])
        DVE(lambda e: e.tensor_reduce(out=ntl, in_=cmpm, axis=AX.X, op=ALU.add), [Bq[1]], [Bq[2]])
        DVE(lambda e: e.tensor_copy(out=cca, in_=ntl), [Bq[2]], [Bq[3]])
        src_, dst_ = cca, ccb
        Bs_, Bd_ = Bq[3], Bq[4]
        for dsh in (1, 2, 4, 8, 16):
            DVE(lambda e, s_=src_, d_=dst_, dsh=dsh: e.tensor_copy(out=d_[:, 0:dsh], in_=s_[:, 0:dsh]), [Bs_], [Bd_])
            DVE(lambda e, s_=src_, d_=dst_, dsh=dsh: e.tensor_tensor(out=d_[:, dsh:32], in0=s_[:, dsh:32], in1=s_[:, 0:32 - dsh], op=ALU.add),
                [Bs_], [Bd_])
            src_, dst_ = dst_, src_
            Bs_, Bd_ = Bd_, Bs_
        tend, Btend = src_, Bs_
        DVE(lambda e: e.tensor_tensor(out=base, in0=tend, in1=ntl, op=ALU.subtract), [Btend, Bq[2]], [Bq[5]])
        DVE(lambda e: e.tensor_scalar(out=base, in0=base, scalar1=128.0, scalar2=None, op0=ALU.mult), [Bq[5]], [Bq[5]])
        DVE(lambda e, brk=brk: e.tensor_tensor(out=val, in0=ps[brk][:, :].rearrange("p (t e) -> p t e", t=NT),
                                      in1=base.unsqueeze(1).to_broadcast([128, NT, 32]), op=ALU.add), [psb[brk], Bq[5]], [Bq[6]])
        for k in range(2):
            DVE(lambda e, k=k: e.tensor_tensor(out=prod, in0=val, in1=OH_all[:, :, k, :], op=ALU.mult), [Bq[6]] + B_oh, [Bq[7]])
            DVE(lambda e, k=k: e.tensor_reduce(out=posf[:, :, k], in_=prod, axis=AX.X, op=ALU.add), [Bq[7]], [Bq[8]])
        DVE(lambda e: e.tensor_copy(out=posu[:].rearrange("p t k -> p (t k)"), in_=posf.rearrange("p t k -> p (t k)")), [Bq[8]], [Bq[9]])
        DVE(lambda e: e.tensor_tensor(out=cmp2, in0=tend.unsqueeze(1).to_broadcast([128, 64, 32]),
                                      in1=sval[:, :].unsqueeze(2).to_broadcast([128, 64, 32]), op=ALU.is_le), [Btend, B_const], [Bq[10]])
        DVE(lambda e: e.tensor_reduce(out=esl, in_=cmp2, axis=AX.X, op=ALU.add), [Bq[10]], [Bq[11]])
        DVE(lambda e: e.tensor_scalar(out=esl, in0=esl, scalar1=31.0, scalar2=128.0, op0=ALU.min, op1=ALU.mult), [Bq[11]], [Bq[11]])
        DVE(lambda e: e.tensor_scalar(out=esl, in0=esl, scalar1=pidx[:, 0:1], scalar2=None, op0=ALU.add), [Bq[11], B_const], [Bq[11]])
        B_idxe = sc.buf()
        DVE(lambda e: e.tensor_copy(out=idxe[:], in_=esl), [Bq[11]], [B_idxe])
        if "posf" in dbg_d:
            sdma(dbg_d["posf"][:, :], posf.rearrange("p t k -> p (t k)"), "dbg", reads=[Bq[8]])
            sdma(dbg_d["esl"][:, :], esl, "dbg", reads=[Bq[11]])
        B_sc = [sc.buf() for _ in range(2 * NT)]
        for t in range(NT):
            for k in range(2):
                sc.dma("pool", (lambda t=t, k=k: (lambda e: e.indirect_dma_start(
                    out=xs_d[:, :], out_offset=bass.IndirectOffsetOnAxis(ap=posu[:, t, k:k + 1], axis=0),
                    in_=x_hi_all[:, t, :], in_offset=None)))(), f"sct{(2 * t + k) % 4}", reads=[B_xhi[t], Bq[9], B_ysg], writes=[B_sc[2 * t + k]])
        sc.barrier()

        NSLOT = 64
        wgu_s = [pages(32, 2).rearrange("p (k c) -> p k c", k=8), pages(34, 2).rearrange("p (k c) -> p k c", k=8)]
        wd_s = [pages(36, 1).rearrange("p (k c) -> p k c", k=2), pages(37, 1).rearrange("p (k c) -> p k c", k=2)]
        xrow = [arena[:, 15 * PAGE:15 * PAGE + 1024], arena[:, 15 * PAGE + 1024:16 * PAGE]]
        xsT = [tmp[:, 0:1024].rearrange("p (k t) -> p k t", k=8), tmp[:, 1024:2048].rearrange("p (k t) -> p k t", k=8)]
        sg_t = [tmp[:, 2048:2304], tmp[:, 2304:2560]]
        h_t = [tmp[:, 2560:2816], tmp[:, 2816:3072]]
        hT_t = [tmp[:, 3072:3328].rearrange("p (j t) -> p j t", j=2), tmp[:, 3328:3584].rearrange("p (j t) -> p j t", j=2)]
        y_sb = [tmpf(2048, 1024), tmpf(3072, 1024)]
        B_wgs, B_wds, B_xr, B_xsT = ([sc.buf(), sc.buf()] for _ in range(4))
        B_sgm, B_hm, B_hTm, B_ysb = ([sc.buf(), sc.buf()] for _ in range(4))
        B_ysw = [sc.buf() for _ in range(NSLOT)]
        gub = Rot([0, 1])
        htb = Rot([2, 3])
        yb = Rot([(4, 5), (6, 7)])
        pend = []
        for s_ in range(NSLOT):
            sb_ = s_ % 2
            sdma(xrow[sb_], xs_d[s_ * 128:(s_ + 1) * 128, :], f"xr{sb_}", reads=B_sc, writes=[B_xr[sb_]])
            sc.dma("pool", (lambda s_=s_, sb_=sb_: (lambda e: e.indirect_dma_start(
                out=wgu_s[sb_].rearrange("p k c -> p (k c)"), out_offset=None, in_=wgu_tab[:, :],
                in_offset=bass.IndirectOffsetOnAxis(ap=idxe[:, s_:s_ + 1], axis=0))))(), f"wgs{sb_}",
                reads=[B_idxe] + B_wbf, writes=[B_wgs[sb_]])
            sc.dma("pool", (lambda s_=s_, sb_=sb_: (lambda e: e.indirect_dma_start(
                out=wd_s[sb_].rearrange("p k c -> p (k c)"), out_offset=None, in_=wd_tab[:, :],
                in_offset=bass.IndirectOffsetOnAxis(ap=idxe[:, s_:s_ + 1], axis=0))))(), f"wds{sb_}",
                reads=[B_idxe] + B_wbf, writes=[B_wds[sb_]])
            bx = htb.next()
            pbx = ps[bx][:, :].bitcast(BF16)
            sc.op("pe", (lambda pbx=pbx, sb_=sb_: (lambda e: [e.transpose(pbx[:, j * 128:(j + 1) * 128], xrow[sb_][:, j * 128:(j + 1) * 128], ident_bf[:, :]) for j in range(8)][-1]))(),
                  [B_xr[sb_], B_const], [psb[bx]])
            ACT(xsT[sb_][:, :, :], pbx[:, :].rearrange("p (k t) -> p k t", k=8), AF.Copy, [psb[bx]], [B_xsT[sb_]])
            bgu = gub.next()
            PE([(ps[bgu][:, :], xsT[sb_][:, kc, :], wgu_s[sb_][:, kc, :], kc == 0, kc == 7) for kc in range(8)],
               [B_xsT[sb_], B_wgs[sb_]], [psb[bgu]])
            ACT(sg_t[sb_][:, :], ps[bgu][:, 0:256], AF.Silu, [psb[bgu]], [B_sgm[sb_]])
            DVE(lambda e, bgu=bgu, sb_=sb_: e.tensor_tensor(out=h_t[sb_][:, :], in0=sg_t[sb_][:, :], in1=ps[bgu][:, 256:512], op=ALU.mult),
                [psb[bgu], B_sgm[sb_]], [B_hm[sb_]])

            def moe_s2(s_=s_, sb_=sb_):
                bh_ = htb.next()
                pb = ps[bh_][:, :].bitcast(BF16)
                sc.op("pe", (lambda sb_=sb_, pb=pb: (lambda e: [e.transpose(pb[:, j * 128:(j + 1) * 128], h_t[sb_][:, j * 128:(j + 1) * 128], ident_bf[:, :]) for j in range(2)][-1]))(),
                      [B_hm[sb_], B_const], [psb[bh_]])
                ACT(hT_t[sb_], pb[:, 0:256].rearrange("p (j t) -> p j t", j=2), AF.Copy, [psb[bh_]], [B_hTm[sb_]])
                by = yb.next()
                for hf in range(2):
                    PE([(ps[by[hf]][:, :], hT_t[sb_][:, j, :], wd_s[sb_][:, j, hf * 512:(hf + 1) * 512], j == 0, j == 1) for j in range(2)],
                       [B_hTm[sb_], B_wds[sb_]], [psb[by[hf]]])
                    if hf == 0:
                        ACT(y_sb[sb_][:, 0:512], ps[by[hf]][:, :], AF.Copy, [psb[by[hf]]], [B_ysb[sb_]])
                    else:
                        DVE(lambda e, sb_=sb_, b=by[hf]: e.tensor_copy(out=y_sb[sb_][:, 512:1024], in_=ps[b][:, :]), [psb[by[hf]]], [B_ysb[sb_]])
                sdma(ys_d[s_ * 128:(s_ + 1) * 128, :], y_sb[sb_][:, :], f"ysw{sb_}", reads=[B_ysb[sb_]], writes=[B_ysw[s_]])
            pend.append(moe_s2)
            while len(pend) > 1:
                pend.pop(0)()
        while pend:
            pend.pop(0)()
        sc.barrier()

        r1 = [tmpf(0, 1024), tmpf(1024, 1024)]
        r2 = [tmpf(2048, 1024), tmpf(3072, 1024)]
        yo = [yit[0], yit[1]]
        B_r1, B_r2, B_yo = ([sc.buf(), sc.buf()] for _ in range(3))
        for t in range(NT):
            tb = t % 2
            sdma(yo[tb][:, :], yinit_d[t * 128:(t + 1) * 128, :], f"yo_in{tb}", reads=[B_yi[t]], writes=[B_yo[tb]])
            for k, (rk, Brk) in enumerate(((r1, B_r1), (r2, B_r2))):
                sc.dma("pool", (lambda t=t, k=k, rk=rk, tb=tb: (lambda e: e.indirect_dma_start(
                    out=rk[tb][:, :], out_offset=None, in_=ys_d[:, :],
                    in_offset=bass.IndirectOffsetOnAxis(ap=posu[:, t, k:k + 1], axis=0))))(), f"rg{k}{tb}",
                    reads=[Bq[9]] + B_ysw, writes=[Brk[tb]])
            DVE(lambda e, t=t, tb=tb: e.scalar_tensor_tensor(out=yo[tb][:, :], in0=r1[tb][:, :], scalar=wk_all[:, t, 0:1], in1=yo[tb][:, :],
                                                               op0=ALU.mult, op1=ALU.add), [B_r1[tb], B_wk[t], B_yo[tb]], [B_yo[tb]])
            DVE(lambda e, t=t, tb=tb: e.scalar_tensor_tensor(out=yo[tb][:, :], in0=r2[tb][:, :], scalar=wk_all[:, t, 1:2], in1=yo[tb][:, :],
                                                               op0=ALU.mult, op1=ALU.add), [B_r2[tb], B_wk[t], B_yo[tb]], [B_yo[tb]])
            layernorm(yo[tb][:, :], B_yo[tb], yo[tb][:, :], B_yo[tb], 2, 3)
            sdma(y_d[seq, t * 128:(t + 1) * 128, :], yo[tb][:, :], f"yout{tb}", reads=[B_yo[tb]], writes=[B_ysg])
        sc.barrier()

    sc.barrier()
    sc.emit()
    es.close()
    return nc


def _consts():
    c = {}
    c["c_ident"] = np.eye(128, dtype=np.float32)
    a = np.arange(128)
    tri = np.zeros((128, 512), np.float32)
    tri[:, 0:128] = np.where(a[:, None] <= a[None, :], 0.0, NEG)
    c["c_tri"] = tri
    anti = np.zeros((128, 512), np.float32)
    anti[:, 384:512] = np.where(a[:, None] > a[None, :], 0.0, NEG)
    c["c_anti"] = anti
    n = np.arange(128)
    t = np.arange(S)
    c["c_cmpmask"] = np.where(n[:, None] * 16 + 31 <= t[None, :], 0.0, NEG).astype(np.float32)
    tt = (np.arange(NT)[None, :, None] * 128 + np.arange(128)[:, None, None])
    j = np.arange(32)[None, None, :]
    cur = tt // 64
    valid = j * 64 <= tt
    forced = (j == 0) | (j == cur) | (j == cur - 1)
    c["c_selconst"] = np.where(valid, 1e4 * forced, -1e4).astype(np.float32).reshape(128, NT * 32)
    cs = np.arange(128) * 16
    ce = cs + 31
    ss = np.arange(32) * 64
    se = ss + 63
    ov = ((cs[:, None] <= se[None, :]) & (ce[:, None] >= ss[None, :])).astype(np.float32)
    ov[127] = 0.0
    c["c_overlap"] = ov
    c["c_eexp"] = (np.arange(S)[None, :] // 64 == np.arange(32)[:, None]).astype(np.float32)
    c["c_ones"] = np.ones((128, 128), np.float32)
    c["c_lstrict"] = (a[:, None] < a[None, :]).astype(np.float32)
    c["c_thr128"] = np.broadcast_to(np.arange(32, dtype=np.float32)[None, :] * 128.0, (128, 32)).copy()
    c["c_sval"] = np.broadcast_to(np.arange(64, dtype=np.float32)[None, :], (128, 64)).copy()
    c["c_pidx"] = np.arange(128, dtype=np.float32).reshape(128, 1)
    return c


def _weights(inp):
    w = {}
    f = lambda a: np.ascontiguousarray(a, dtype=np.float32)
    w["w_in"] = f(inp["w_in"][0])
    w["peT"] = f(np.transpose(inp["cmp_pe"][0], (0, 2, 1)))
    w["cmp_w1"] = f(inp["cmp_w1"][0])
    w["cmp_b1"] = f(np.transpose(inp["cmp_b1"][0], (1, 0)))
    w["cmp_w2"] = f(inp["cmp_w2"][0])
    w["cmp_b2k"] = f(inp["cmp_b2"][0, 0].reshape(64, 1))
    w["cmp_b2v"] = f(inp["cmp_b2"][0, 1].reshape(1, 64))
    w["conv_wT"] = f(np.transpose(inp["conv_w"][0], (1, 0)).reshape(4, 128, 3))
    w["w_nsa_out"] = f(inp["w_nsa_out"][0])
    w["w_conv_out"] = f(inp["w_conv_out"][0])
    w["w_o"] = f(inp["w_o"][0])
    for k in ("ln1_g", "ln1_b", "ln2_g", "ln2_b", "ple_gate_b"):
        w[k] = f(inp[k][0].reshape(1, D))
    w["w_router"] = f(np.concatenate([inp["router_group_w"][0], inp["router_expert_w"][0]], axis=1))
    w["b_router"] = f(np.concatenate([inp["router_group_b"][0], inp["router_expert_b"][0]], axis=0).reshape(1, 36))
    w["expert_w_gate"] = f(inp["expert_w_gate"][0])
    w["expert_w_up"] = f(inp["expert_w_up"][0])
    w["expert_w_down"] = f(inp["expert_w_down"][0])
    w["ple_proj"] = f(inp["ple_proj"][0])
    w["ple_gate_w"] = f(inp["ple_gate_w"][0])
    return w


def kernel(**inputs):
    inp = {k: np.asarray(v) for k, v in inputs.items()}
    x = inp["x"]
    p = inp["p"][0]
    B = x.shape[0]
    nseq = B // NCORES
    shared = _weights(inp)
    shared.update(_consts())
    in_maps = []
    for c in range(NCORES):
        xs = x[c * nseq:(c + 1) * nseq]
        m = dict(shared)
        m["x"] = np.ascontiguousarray(xs)
        m["xT"] = np.ascontiguousarray(np.transpose(xs, (0, 2, 1)))
        m["pT"] = np.ascontiguousarray(np.transpose(p[c * nseq:(c + 1) * nseq], (0, 2, 1)))
        in_maps.append(m)
    nc = build(nseq)
    res = run_bass_kernel_spmd(nc, in_maps, core_ids=list(range(NCORES)))
    out = np.concatenate([np.asarray(r["y"]) for r in res.results], axis=0)
    return out.astype(np.float32, copy=False)
```
